# Optimizing a Trainium2 kernel written in Bass

```python
import jax, jax.numpy as jnp
from jax import lax
import numpy as np

D_MODEL = 2048
BATCH = 4
SEQ = 4096
DEPTH = 2

GRID_W = 64
CTX_LEN = 256
N_MIXERS = 2
MLSTM_HEADS = 8
MLSTM_DV = D_MODEL // MLSTM_HEADS
MLSTM_DK = MLSTM_DV // 2
MLSTM_CHUNK = 64
MLSTM_STATE_COLS = 2 * MLSTM_HEADS * MLSTM_DK + MLSTM_HEADS * MLSTM_DV + 4 * MLSTM_HEADS
MLSTM_IN_COLS = MLSTM_STATE_COLS + MLSTM_HEADS * MLSTM_DV
CONV_WIDTH = 3
D_FF = (((8 * D_MODEL) // 3 + 127) // 128) * 128
N_MOD = 9
RMS_EPS = 1e-6
M_INIT = -1e30

kernel_name = "hybrid_mlstm_shortconv_macaron_dit"


def _rmsnorm(x, g):
    xf = x.astype(jnp.float32)
    y = xf * lax.rsqrt(jnp.mean(xf * xf, axis=-1, keepdims=True) + RMS_EPS)
    return (y * g.astype(jnp.float32)).astype(x.dtype)


def _modulate(x, g, shift, scale):
    return _rmsnorm(x, g) * (1.0 + scale) + shift


def _mod_parts(mod, j):
    return mod[..., 3 * j, :], mod[..., 3 * j + 1, :], mod[..., 3 * j + 2, :]


def _swiglu(h, w_in, w_out):
    g, u = jnp.split(h @ w_in, 2, axis=-1)
    return (jax.nn.silu(g) * u) @ w_out


def _mlstm_split(proj, b_gate):
    B, T, cols = proj.shape
    H, DK, DV = MLSTM_HEADS, MLSTM_DK, MLSTM_DV
    heads = lambda a, d: a.reshape(B, T, H, d).transpose(0, 2, 1, 3)
    o0, o1, o2, o3 = H * DK, 2 * H * DK, 2 * H * DK + H * DV, MLSTM_STATE_COLS
    q = heads(proj[..., :o0], DK) * (DK ** -0.5)
    k = heads(proj[..., o0:o1], DK)
    v = heads(proj[..., o1:o2], DV)
    gates = (proj[..., o2:o3] + b_gate.astype(jnp.float32)).reshape(B, T, 4, H).transpose(2, 0, 3, 1)
    o = proj[..., o3:] if cols == MLSTM_IN_COLS else None
    return q, k, v, gates, o


def _mlstm_scan(q, k, v, log_i, log_f, state0, return_h):
    B, H, T, DK = q.shape
    DV = v.shape[-1]
    L = MLSTM_CHUNK
    nc = T // L

    def chunks(a):
        return jnp.moveaxis(a.reshape(a.shape[:2] + (nc, L) + a.shape[3:]), 2, 0)

    xs = (chunks(q), chunks(k), chunks(v), chunks(log_i), chunks(log_f))
    causal = jnp.tril(jnp.ones((L, L), dtype=bool))

    def step(carry, xc):
        C, n, m = carry
        qc, kc, vc, ic, fc = xc
        b = jnp.cumsum(fc, axis=-1)
        b_end = b[..., -1]
        w_end = b_end[..., None] - b + ic
        m_new = jnp.maximum(b_end + m, jnp.max(w_end, axis=-1))
        carry_decay = jnp.exp(b_end + m - m_new)
        w = jnp.exp(w_end - m_new[..., None])[..., None]
        C_new = carry_decay[..., None, None] * C + jnp.einsum('bhld,bhle->bhde', kc * w, vc)
        n_new = carry_decay[..., None] * n + jnp.sum(kc * w, axis=2)
        if not return_h:
            return (C_new, n_new, m_new), None
        d_log = jnp.where(causal, b[..., :, None] - b[..., None, :] + ic[..., None, :], -jnp.inf)
        inter = b + m[..., None]
        m_out = jnp.maximum(inter, jnp.max(d_log, axis=-1))
        s = jnp.einsum('bhsd,bhjd->bhsj', qc, kc) * jnp.exp(d_log - m_out[..., None])
        w_inter = jnp.exp(inter - m_out)[..., None]
        num = jnp.einsum('bhsj,bhje->bhse', s, vc) + w_inter * jnp.einsum('bhsd,bhde->bhse', qc, C)
        den = jnp.sum(s, axis=-1, keepdims=True) + w_inter * jnp.einsum('bhsd,bhd->bhs', qc, n)[..., None]
        h = num / jnp.maximum(jnp.abs(den), jnp.exp(-m_out)[..., None])
        return (C_new, n_new, m_new), h

    state, hs = lax.scan(step, state0, xs)
    if not return_h:
        return None, state
    return jnp.moveaxis(hs, 0, 2).reshape(B, H, T, DV), state


def _mlstm_bidir(q, k, v, gates, state_f0, state_b0, return_h):
    i_f, f_f = gates[0], jax.nn.log_sigmoid(gates[1])
    i_b, f_b = gates[2], jax.nn.log_sigmoid(gates[3])
    flip = lambda a: jnp.flip(a, axis=2)
    h_f, st_f = _mlstm_scan(q, k, v, i_f, f_f, state_f0, return_h)
    h_b, st_b = _mlstm_scan(flip(q), flip(k), flip(v), flip(i_b), flip(f_b), state_b0, return_h)
    h = h_f + flip(h_b) if return_h else None
    return h, st_f, st_b


def _mlstm_out(h, o, norm_g, w_out, dtype):
    B, H, T, DV = h.shape
    h = h.transpose(0, 2, 1, 3)
    hn = h * lax.rsqrt(jnp.mean(h * h, axis=-1, keepdims=True) + RMS_EPS)
    hn = hn * norm_g.astype(jnp.float32).reshape(H, DV)
    y = hn * jax.nn.sigmoid(o.reshape(B, T, H, DV))
    return y.reshape(B, T, H * DV).astype(dtype) @ w_out


def _zero_state(batch):
    H, DK, DV = MLSTM_HEADS, MLSTM_DK, MLSTM_DV
    return (jnp.zeros((batch, H, DK, DV), jnp.float32),
            jnp.zeros((batch, H, DK), jnp.float32),
            jnp.full((batch, H), M_INIT, jnp.float32))


def _conv3(z, w):
    pad = [(0, 0)] * (z.ndim - 2) + [(1, 1), (0, 0)]
    zp = jnp.pad(z, pad)
    return w[0] * zp[..., :-2, :] + w[1] * zp[..., 1:-1, :] + w[2] * zp[..., 2:, :]


def _short_conv(h, w_in, w_conv, w_out, on_grid):
    bg, cg, u = jnp.split(h @ w_in, 3, axis=-1)
    z = cg * u
    if on_grid:
        B, T, D = z.shape
        rows = T // GRID_W
        zc = _conv3(z.reshape(B, rows, GRID_W, D), w_conv).reshape(B, T, D)
    else:
        zc = _conv3(z, w_conv)
    return (bg * zc) @ w_out


def setup_inputs(seed: int = 0) -> dict:
    key = jax.random.key(seed)
    ks = jax.random.split(key, 24)
    D, H = D_MODEL, MLSTM_HEADS
    n_a = len(range(0, DEPTH, N_MIXERS))
    n_b = len(range(1, DEPTH, N_MIXERS))
    nrm = lambda k, shape, fan_in: jax.random.normal(k, shape, jnp.float32) * (fan_in ** -0.5)
    i_bias = 0.5 * jax.random.normal(ks[10], (n_a, 2, H), jnp.float32)
    f_bias = jax.random.uniform(ks[11], (n_a, 2, H), jnp.float32, minval=3.0, maxval=6.0)
    b_gate = jnp.stack([i_bias[:, 0], f_bias[:, 0], i_bias[:, 1], f_bias[:, 1]], axis=1).reshape(n_a, 4 * H)
    return {
        "x": jax.random.normal(ks[0], (BATCH, SEQ, D), jnp.float32),
        "c": jax.random.normal(ks[1], (BATCH, D), jnp.float32),
        "ctx": jax.random.normal(ks[2], (BATCH, CTX_LEN, D), jnp.float32),
        "c_ctx": jax.random.normal(ks[3], (D,), jnp.float32),
        "w_mod": nrm(ks[4], (DEPTH, D, N_MOD * D), D),
        "b_mod": 0.02 * jax.random.normal(ks[5], (DEPTH, N_MOD * D), jnp.float32),
        "norm_g": 1.0 + 0.1 * jax.random.normal(ks[6], (DEPTH, 3, D), jnp.float32),
        "ffn_w_in": nrm(ks[7], (DEPTH, 2, D, 2 * D_FF), D),
        "ffn_w_out": nrm(ks[8], (DEPTH, 2, D_FF, D), D_FF),
        "mlstm_w_in": nrm(ks[9], (n_a, D, MLSTM_IN_COLS), D),
        "mlstm_b_gate": b_gate,
        "mlstm_norm_g": 1.0 + 0.1 * jax.random.normal(ks[12], (n_a, H * MLSTM_DV), jnp.float32),
        "mlstm_w_out": nrm(ks[13], (n_a, H * MLSTM_DV, D), H * MLSTM_DV),
        "conv_w_in": nrm(ks[14], (n_b, D, 3 * D), D),
        "conv_w": nrm(ks[15], (n_b, CONV_WIDTH, D), CONV_WIDTH),
        "conv_w_out": nrm(ks[16], (n_b, D, D), D),
        "final_norm_g": 1.0 + 0.1 * jax.random.normal(ks[17], (D,), jnp.float32),
    }


def reference(x, c, ctx, c_ctx, w_mod, b_mod, norm_g, ffn_w_in, ffn_w_out,
              mlstm_w_in, mlstm_b_gate, mlstm_norm_g, mlstm_w_out,
              conv_w_in, conv_w, conv_w_out, final_norm_g):
    batch = x.shape[0]
    for i in range(DEPTH):
        kind = i % N_MIXERS
        slot = i // N_MIXERS
        ctx_after = any(j % N_MIXERS == 0 for j in range(i + 1, DEPTH))
        ctx_here = (kind == 0) or ctx_after
        mod = (jax.nn.silu(c) @ w_mod[i] + b_mod[i]).reshape(batch, 1, N_MOD, D_MODEL)
        if ctx_here:
            mod_c = (jax.nn.silu(c_ctx) @ w_mod[i] + b_mod[i]).reshape(N_MOD, D_MODEL)

        sh, sc, g = _mod_parts(mod, 0)
        x = x + 0.5 * g * _swiglu(_modulate(x, norm_g[i, 0], sh, sc), ffn_w_in[i, 0], ffn_w_out[i, 0])
        if ctx_here:
            sh_c, sc_c, g_c = _mod_parts(mod_c, 0)
            ctx = ctx + 0.5 * g_c * _swiglu(_modulate(ctx, norm_g[i, 0], sh_c, sc_c), ffn_w_in[i, 0], ffn_w_out[i, 0])

        sh, sc, g = _mod_parts(mod, 1)
        hx = _modulate(x, norm_g[i, 1], sh, sc)
        if ctx_here:
            sh_c, sc_c, g_c = _mod_parts(mod_c, 1)
            hc = _modulate(ctx, norm_g[i, 1], sh_c, sc_c)
        if kind == 0:
            w_in = mlstm_w_in[slot]
            c_cols = MLSTM_IN_COLS if ctx_after else MLSTM_STATE_COLS
            q_c, k_c, v_c, gt_c, o_c = _mlstm_split((hc @ w_in[:, :c_cols]).astype(jnp.float32), mlstm_b_gate[slot])
            h_c, st_f, st_b = _mlstm_bidir(q_c, k_c, v_c, gt_c, _zero_state(batch), _zero_state(batch), ctx_after)
            q_x, k_x, v_x, gt_x, o_x = _mlstm_split((hx @ w_in).astype(jnp.float32), mlstm_b_gate[slot])
            h_x, _, _ = _mlstm_bidir(q_x, k_x, v_x, gt_x, st_f, st_b, True)
            x = x + g * _mlstm_out(h_x, o_x, mlstm_norm_g[slot], mlstm_w_out[slot], x.dtype)
            if ctx_after:
                ctx = ctx + g_c * _mlstm_out(h_c, o_c, mlstm_norm_g[slot], mlstm_w_out[slot], ctx.dtype)
        else:
            x = x + g * _short_conv(hx, conv_w_in[slot], conv_w[slot], conv_w_out[slot], True)
            if ctx_after:
                ctx = ctx + g_c * _short_conv(hc, conv_w_in[slot], conv_w[slot], conv_w_out[slot], False)

        sh, sc, g = _mod_parts(mod, 2)
        x = x + 0.5 * g * _swiglu(_modulate(x, norm_g[i, 2], sh, sc), ffn_w_in[i, 1], ffn_w_out[i, 1])
        if ctx_after:
            sh_c, sc_c, g_c = _mod_parts(mod_c, 2)
            ctx = ctx + 0.5 * g_c * _swiglu(_modulate(ctx, norm_g[i, 2], sh_c, sc_c), ffn_w_in[i, 1], ffn_w_out[i, 1])
    return _rmsnorm(x, final_norm_g)
```

```python
import numpy as np
from contextlib import ExitStack
import concourse.bass as bass
import concourse.mybir as mybir
from concourse.bass_utils import run_bass_kernel_spmd

F32 = mybir.dt.float32
BF16 = mybir.dt.bfloat16
ALU = mybir.AluOpType
AF = mybir.ActivationFunctionType
AX = mybir.AxisListType

D = 2048
NKC = 16
DFF = 5504
NFC = 43
H = 8
DK = 128
DV = 256
DVE_ = 257
TOK = 2048
NT = 512
NCH = 16
CTX = 256
EPS = 1e-6
WSLOT = 8192
NWS = 3
STOP_AT = 99
FUSED = True
USE_CC = True
WCACHE = True


class Buf:
    __slots__ = ("name", "last_w", "readers")

    def __init__(self, name):
        self.name = name
        self.last_w = None
        self.readers = []


class Op:
    __slots__ = ("idx", "eng", "stream", "pos", "fn", "deps", "signal", "value", "is_dma", "inc")


class Prog:
    ENGS = ("pe", "dve", "act", "pool", "sp")

    def __init__(self, nc):
        self.nc = nc
        self.ops = []
        self.stream_ops = {}

    def add(self, eng, fn, reads=(), writes=(), dma=None, inc=None):
        op = Op()
        op.inc = inc if inc is not None else (16 if dma is not None else 1)
        op.idx = len(self.ops)
        op.eng = eng
        op.stream = ("dma:" + dma) if dma else eng
        op.fn = fn
        op.is_dma = dma is not None
        op.signal = op.is_dma
        op.value = 0
        deps = set()
        for b in reads:
            if b.last_w is not None:
                deps.add(b.last_w)
        for b in writes:
            if b.last_w is not None:
                deps.add(b.last_w)
            deps.update(b.readers)
        deps.discard(op.idx)
        if eng == "pe" and not op.is_dma:
            deps = {d for d in deps if self.ops[d].stream != "pe"}
        op.deps = deps
        lst = self.stream_ops.setdefault(op.stream, [])
        op.pos = len(lst)
        lst.append(op.idx)
        self.ops.append(op)
        wset = set(id(b) for b in writes)
        for b in reads:
            if id(b) in wset:
                continue
            b.readers = [r for r in b.readers if self.ops[r].stream != op.stream]
            b.readers.append(op.idx)
        for b in writes:
            b.last_w = op.idx
            b.readers = []
        return op

    def emit(self):
        nc = self.nc
        ops = self.ops
        seen = {e: {} for e in self.ENGS}
        waits = [[] for _ in ops]
        for op in ops:
            need = {}
            for d in op.deps:
                P = ops[d]
                if need.get(P.stream, -1) < P.pos:
                    need[P.stream] = P.pos
            sE = seen[op.eng]
            for S, pos in need.items():
                if sE.get(S, -1) >= pos:
                    continue
                sE[S] = pos
                prod = ops[self.stream_ops[S][pos]]
                prod.signal = True
                waits[op.idx].append(prod.idx)
        for S, lst in self.stream_ops.items():
            cnt = 0
            for i in lst:
                if ops[i].signal:
                    cnt += ops[i].inc
                    ops[i].value = cnt
        with ExitStack() as es:
            sems = {}
            for S in self.stream_ops:
                sems[S] = es.enter_context(nc.semaphore("s_" + S.replace(":", "_")))
            block = es.enter_context(nc.Block())

            def run(engkey):
                def body(eng):
                    for op in ops:
                        if op.eng != engkey:
                            continue
                        for w in waits[op.idx]:
                            P = ops[w]
                            eng.wait_ge(sems[P.stream], P.value)
                        if op.fn is None:
                            continue
                        ins = op.fn(eng)
                        if op.signal:
                            ins.then_inc(sems[op.stream], op.inc)
                return body

            block.tensor(run("pe"))
            block.vector(run("dve"))
            block.scalar(run("act"))
            block.gpsimd(run("pool"))
            block.sync(run("sp"))


class Builder:
    def __init__(self, mode, dbg=None):
        self.mode = mode
        self.dbg = dbg or {}
        self.nc = bass.Bass("TRN2", target_bir_lowering=False)
        self.P = Prog(self.nc)
        self.es = ExitStack()
        self.nbuf = 0
        self.cur = None

    def sb(self, name, shape, dt=F32):
        self.nbuf += 1
        return (self.cur or self.es).enter_context(self.nc.sbuf_tensor(f"sb{self.nbuf}_" + name, list(shape), dt))

    def dram(self, name, shape, dt, kind):
        return self.nc.dram_tensor(name, list(shape), dt, kind=kind).ap()

    def din(self, name, shape, dt=F32):
        return self.dram(name, shape, dt, "ExternalInput")

    def dout(self, name, shape, dt=F32):
        return self.dram(name, shape, dt, "ExternalOutput")

    def scratch(self, name, shape, dt, produced_in):
        if self.mode == "F":
            return self.dram(name, shape, dt, "Internal")
        if self.mode == produced_in:
            return self.dout(name, shape, dt)
        return self.din(name, shape, dt)

    def B(self, name):
        self.nbuf += 1
        return Buf(name)

    def pe(self, fn, r=(), w=()):
        return self.P.add("pe", fn, r, w)

    def dve(self, fn, r=(), w=()):
        return self.P.add("dve", fn, r, w)

    def act(self, fn, r=(), w=()):
        return self.P.add("act", fn, r, w)

    def dma(self, out, in_, r=(), w=(), q="sp", stream=None, slow=False):
        assert stream is not None
        if slow:
            return self.P.add(q, lambda e: e.dma_start(out=out, in_=in_, allow_slow_non_contiguous=True), r, w, dma=stream)
        return self.P.add(q, lambda e: e.dma_start(out=out, in_=in_), r, w, dma=stream)

    def setup_common(self):
        nc = self.nc
        self.wslots = [self.sb(f"wslot{i}", [128, WSLOT], BF16) for i in range(NWS)]
        self.wbufs = [self.B(f"wslot{i}") for i in range(NWS)]
        self.wi = 0
        self.psum = [self.es.enter_context(nc.psum_tensor(f"ps{i}", [128, 512], F32)) for i in range(8)]
        self.psb = [self.B(f"ps{i}") for i in range(8)]
        self.pi = 0
        self.stats_ready = None
        self.bg_gen = None
        self.ones_bf = self.sb("ones_bf", [128, 128], BF16)
        self.ones_f = self.sb("ones_f", [128, 128], F32)
        self.b_const = self.B("const")
        self.dve(lambda e: e.memset(self.ones_bf[:], 1.0), w=[self.b_const])
        self.dve(lambda e: e.memset(self.ones_f[:], 1.0), w=[self.b_const])
        self.eps_t = self.sb("eps_t", [128, 1], F32)
        self.dve(lambda e: e.memset(self.eps_t[:], EPS), w=[self.b_const])
        self.one_t = self.sb("one_t", [128, 1], F32)
        self.dve(lambda e: e.memset(self.one_t[:], 1.0), w=[self.b_const])
        self.zero_t = self.sb("zero_t", [128, 1], F32)
        self.dve(lambda e: e.memset(self.zero_t[:], 0.0), w=[self.b_const])

    def mk_cache(self, name, ntiles, nel):
        return dict(ap=self.dram(name, [ntiles, 128, nel], BF16, "Internal"),
                    bufs=[self.B(f"{name}{j}") for j in range(ntiles)], done=[False] * ntiles)

    def wload(self, src, nel, cache=None, j=None):
        i = self.wi % NWS
        self.wi += 1
        slot, buf = self.wslots[i], self.wbufs[i]
        if cache is not None and cache["done"][j]:
            self.dma(slot[:, 0:nel], cache["ap"][j], r=[cache["bufs"][j]], w=[buf], q="pool", stream=f"w{i}")
            return slot, buf
        self.dma(slot[:, 0:nel], src, w=[buf], q="pool", stream=f"w{i}")
        if cache is not None:
            self.dma(cache["ap"][j], slot[:, 0:nel], r=[buf], w=[cache["bufs"][j]], q="sp", stream=f"wc{i}")
            cache["done"][j] = True
        return slot, buf

    def bank(self):
        i = self.pi % 6
        self.pi += 1
        return self.psum[i], self.psb[i]

    def mod_alloc(self, l):
        return dict(cs=self.sb(f"c_sb{l}", [128, NKC, 2], F32), csil=self.sb(f"c_sil{l}", [128, NKC, 2], BF16),
                    bm=self.sb(f"bm{l}", [128, 144], F32))

    def mod_phase_gen(self, l, wmod, bmodT, cT, modT, bmod_buf, tl):
        cs, csil, bm = tl["cs"], tl["csil"], tl["bm"]
        csb = self.B("c_sb")
        self.dma(cs[:], cT, w=[csb], stream=f"cT{l}")
        csilb = self.B("csil")
        self.act(lambda e: e.activation(out=csil[:], in_=cs[:], func=AF.Silu), r=[csb], w=[csilb])
        bmb = self.B("bm")
        self.dma(bm[:], bmodT, w=[bmb], stream=f"bm{l}")
        ps, psb = self.psum[6], self.psb[6]
        psv = ps[:, 0:288].rearrange("p (j k) -> p j k", k=2)
        for t in range(36):
            slot, sbuf_ = self.wload(wmod[t // 18][t % 18], NKC * 512)
            sv = slot[:, 0:NKC * 512].rearrange("p (k n) -> p k n", n=512)
            for j in range(4):
                col = t * 4 + j
                for kc in range(NKC):
                    self.pe(lambda e, col=col, kc=kc, j=j, sv=sv: e.matmul(
                        psv[:, col, :], lhsT=sv[:, kc, j * 128:(j + 1) * 128], rhs=csil[:, kc, :],
                        start=(kc == 0), stop=(kc == NKC - 1)),
                        r=[sbuf_, csilb], w=[psb])
            if t < 35:
                yield
        for col in range(2):
            self.dve(lambda e, col=col: e.tensor_tensor(out=modT[:, :, col], in0=psv[:, :, col], in1=bm[:],
                                                        op=ALU.add), r=[psb, bmb], w=[bmod_buf])

    def mod_phase(self, l, wmod, bmodT, cT, modT, bmod_buf, tl=None):
        for _ in self.mod_phase_gen(l, wmod, bmodT, cT, modT, bmod_buf, tl or self.mod_alloc(l)):
            pass

    def mod_consts_alloc(self, l, tag):
        return [(self.sb(f"A_{tag}_{l}_{sl}", [128, NKC], F32), self.sb(f"G_{tag}_{l}_{sl}", [128, NKC], F32),
                 self.sb(f"S_{tag}_{l}_{sl}", [128, NKC], F32)) for sl in range(3)]

    def mod_consts(self, l, modT, modb, normgT, ngb, col, tag, tiles=None):
        res = []
        tiles = tiles or self.mod_consts_alloc(l, tag)
        for sl in range(3):
            A, G, Sh = tiles[sl]
            b = self.B("modc")
            sc = modT[:, (3 * sl + 1) * 16:(3 * sl + 2) * 16, col]
            gt = modT[:, (3 * sl + 2) * 16:(3 * sl + 3) * 16, col]
            ng = normgT[:, (l * 3 + sl) * 16:(l * 3 + sl + 1) * 16]
            self.dve(lambda e, A=A, sc=sc, ng=ng: e.scalar_tensor_tensor(
                out=A[:], in0=sc, scalar=1.0, in1=ng, op0=ALU.add, op1=ALU.mult), r=[modb, ngb], w=[b])
            gs = 0.5 if sl != 1 else 1.0
            self.dve(lambda e, G=G, gt=gt, gs=gs: e.tensor_scalar(
                out=G[:], in0=gt, scalar1=gs, scalar2=None, op0=ALU.mult), r=[modb], w=[b])
            sh = modT[:, (3 * sl) * 16:(3 * sl + 1) * 16, col]
            self.dve(lambda e, Sh=Sh, sh=sh: e.tensor_copy(out=Sh[:], in_=sh), r=[modb], w=[b])
            res.append((A, Sh, G, b))
        return res

    def alloc_tile_state(self):
        self.xT = self.sb("xT", [128, NKC, NT], F32)
        self.xb = [self.B(f"xT{c}") for c in range(NKC)]
        self.hT = self.sb("hT", [128, NKC, NT], BF16)
        self.hb = [self.B(f"hT{c}") for c in range(NKC)]
        self.R = self.sb("R", [128, NFC, NT], BF16)
        self.Rb = [self.B(f"R{c}") for c in range(NFC)]
        self.sq = [self.sb(f"sq{i}", [128, NT], BF16) for i in range(2)]
        self.sqb = [self.B(f"sq{i}") for i in range(2)]
        self.tmp = [self.sb(f"tmp{i}", [128, NT], F32) for i in range(3)]
        self.tmpb = [self.B(f"tmp{i}") for i in range(3)]
        self.ti = 0
        self.rstd = self.sb("rstd", [128, NT], F32)
        self.rstdb = self.B("rstd")
        self.sqr = self.sb("sqr", [128, NT], F32)
        self.sqrb = self.B("sqr")

    def gettmp(self):
        i = self.ti % 3
        self.ti += 1
        return self.tmp[i], self.tmpb[i]

    def stat_square(self, c, W):
        sq, sqb = self.sq[c % 2], self.sqb[c % 2]
        xT = self.xT
        self.act(lambda e, c=c, sq=sq, xT=xT: e.activation(out=sq[:, :W], in_=xT[:, c, :W], func=AF.Square),
                 r=[self.xb[c]], w=[sqb])
        return (c, sq, sqb)

    def stat_accum(self, pend, W):
        c, sq, sqb = pend
        ps, psb = self.psum[7], self.psb[7]
        self.pe(lambda e, c=c, sq=sq: e.matmul(ps[:, :W], lhsT=self.ones_bf[:], rhs=sq[:, :W],
                                              start=(c == 0), stop=(c == NKC - 1)),
                r=[sqb, self.b_const], w=[psb])

    def rms_stats(self, W):
        xT = self.xT
        ps, psb = self.psum[7], self.psb[7]
        if self.stats_ready == W:
            self.stats_ready = None
        else:
            for c in range(NKC):
                self.stat_accum(self.stat_square(c, W), W)
        sqr, rstd = self.sqr, self.rstd
        self.act(lambda e: e.activation(out=sqr[:, :W], in_=ps[:, :W], func=AF.Sqrt,
                                        bias=self.eps_t[:], scale=1.0 / D),
                 r=[psb, self.b_const], w=[self.sqrb])
        self.dve(lambda e: e.reciprocal(out=rstd[:, :W], in_=sqr[:, :W]), r=[self.sqrb], w=[self.rstdb])

    def modulate(self, W, mc):
        A, Sh, G, mb = mc
        xT, hT = self.xT, self.hT
        rstd = self.rstd
        for c in range(NKC):
            t, tb = self.gettmp()
            self.dve(lambda e, c=c, t=t: e.scalar_tensor_tensor(
                out=t[:, :W], in0=xT[:, c, :W], scalar=A[:, c:c + 1], in1=rstd[:, :W],
                op0=ALU.mult, op1=ALU.mult), r=[self.xb[c], mb, self.rstdb], w=[tb])
            self.act(lambda e, c=c, t=t: e.activation(out=hT[:, c, :W], in_=t[:, :W], func=AF.Identity,
                                                      bias=Sh[:, c:c + 1], scale=1.0),
                     r=[tb, mb], w=[self.hb[c]])

    def ffn(self, W, mc, w_in, w_out, post_stats=True, c_in=None, c_out=None):
        A, Sh, G, mb = mc
        xT, hT, R = self.xT, self.hT, self.R
        self.rms_stats(W)
        self.modulate(W, mc)
        for i in range(NFC):
            slot, sbuf_ = self.wload(w_in[i], NKC * 256, c_in, i)
            sv = slot[:, 0:NKC * 256].rearrange("p (k n) -> p k n", n=256)
            pg, pgb = self.bank()
            pu, pub = self.bank()
            for half, (ps, psb) in enumerate(((pg, pgb), (pu, pub))):
                for kc in range(NKC):
                    self.pe(lambda e, ps=ps, sv=sv, half=half, kc=kc: e.matmul(
                        ps[:, :W], lhsT=sv[:, kc, half * 128:(half + 1) * 128], rhs=hT[:, kc, :W],
                        start=(kc == 0), stop=(kc == NKC - 1)), r=[sbuf_, self.hb[kc]], w=[psb])
            t, tb = self.gettmp()
            self.act(lambda e, t=t, pg=pg: e.activation(out=t[:, :W], in_=pg[:, :W], func=AF.Silu),
                     r=[pgb], w=[tb])
            self.dve(lambda e, t=t, pu=pu, i=i: e.tensor_tensor(out=R[:, i, :W], in0=t[:, :W], in1=pu[:, :W],
                                                              op=ALU.mult), r=[tb, pub], w=[self.Rb[i]])
        pend = None
        for m in range(NKC):
            slot, sbuf_ = self.wload(w_out[m], NFC * 128, c_out, m)
            sv = slot[:, 0:NFC * 128].rearrange("p (k n) -> p k n", n=128)
            ps, psb = self.bank()
            for k in range(NFC):
                self.pe(lambda e, ps=ps, sv=sv, k=k: e.matmul(
                    ps[:, :W], lhsT=sv[:, k, :], rhs=R[:, k, :W], start=(k == 0), stop=(k == NFC - 1)),
                    r=[sbuf_, self.Rb[k]], w=[psb])
            if pend is not None:
                self.stat_accum(pend, W)
                pend = None
            self.dve(lambda e, ps=ps, m=m: e.scalar_tensor_tensor(
                out=xT[:, m, :W], in0=ps[:, :W], scalar=G[:, m:m + 1], in1=xT[:, m, :W],
                op0=ALU.mult, op1=ALU.add), r=[psb, mb, self.xb[m]], w=[self.xb[m]])
            if post_stats:
                pend = self.stat_square(m, W)
        if pend is not None:
            self.stat_accum(pend, W)
        if post_stats:
            self.stats_ready = W

    def load_xT(self, src, W):
        self.dma(self.xT[:, :, :W], src.rearrange("c p t -> p c t"), w=self.xb, stream="xTld")

    def store_xT(self, dst, W, wb=()):
        self.dma(dst.rearrange("c p t -> p c t"), self.xT[:, :, :W], r=self.xb, w=list(wb), stream="xTst")

    def finish(self, out_ops_bufs):
        self.P.add("sp", None, reads=out_ops_bufs, writes=())
        self.P.emit()
        self.es.close()
        return self.nc


def _tiles_cols(w, col_lists):
    K = w.shape[0]
    kc = K // 128
    w3 = w.reshape(kc, 128, w.shape[1])
    out = []
    for cols in col_lists:
        t = w3[:, :, cols]
        out.append(np.ascontiguousarray(t.transpose(1, 0, 2)).reshape(128, -1))
    return np.stack(out, 0)


def _vecT(v):
    return np.ascontiguousarray(v.reshape(-1, 128).T)


def prep_shared(inp):
    sh = {}
    sh["wmod"] = [_tiles_cols(inp["w_mod"][l], [np.arange(t * 512, (t + 1) * 512) for t in range(36)])
                  for l in range(2)]
    sh["bmodT"] = [_vecT(inp["b_mod"][l]) for l in range(2)]
    sh["normgT"] = np.concatenate([_vecT(inp["norm_g"][l, s]) for l in range(2) for s in range(3)], axis=1)
    sh["fnormgT"] = _vecT(inp["final_norm_g"])
    ffn_in, ffn_out = [], []
    for l in range(2):
        for j in range(2):
            wi = inp["ffn_w_in"][l, j]
            ffn_in.append(_tiles_cols(wi, [np.concatenate([np.arange(i * 128, (i + 1) * 128),
                                                           DFF + np.arange(i * 128, (i + 1) * 128)])
                                           for i in range(NFC)]))
            wo = inp["ffn_w_out"][l, j]
            ffn_out.append(_tiles_cols(wo, [np.arange(m * 128, (m + 1) * 128) for m in range(NKC)]))
    sh["ffn_in"] = ffn_in
    sh["ffn_out"] = ffn_out
    return sh


def _barrier(self):
    lasts = []
    for lst in self.P.stream_ops.values():
        for i in reversed(lst):
            if self.P.ops[i].fn is not None:
                lasts.append(i)
                break
    for e in Prog.ENGS:
        op = self.P.add(e, None)
        op.deps = set(lasts)


def _Rbufs(self, off, n):
    return self.Rb[off // NT:(off + n + NT - 1) // NT]


def _mlstm_proj(self, W, mc, c0, dd, kind):
    nsub = W // 128
    hT = self.hT
    is_ctx = kind != "own"
    self.rms_stats(W)
    self.modulate(W, mc)
    Rf = self.R[:].rearrange("p c t -> p (c t)")
    qst = Rf[:, 0:4096].rearrange("p (s h j) -> p s h j", s=4, h=8)
    kst = Rf[:, 4096:8192].rearrange("p (s h j) -> p s h j", s=4, h=8)
    ktm = Rf[:, 8192:12288].rearrange("p (s n) -> p s n", s=4)
    osg = Rf[:, 12288:20480].rearrange("p (s n) -> p s n", s=4)
    qb, kb, ktb, ob = self.Rbufs(0, 4096), self.Rbufs(4096, 4096), self.Rbufs(8192, 4096), self.Rbufs(12288, 8192)
    Vst, Vb = self.Vst, self.Vstb
    if not is_ctx:
        for i in range(8):
            slot, sbuf_ = self.wload(dd["wqk"][i], NKC * 256, dd.get("c_wqk"), i)
            sv = slot[:, 0:NKC * 256].rearrange("p (k n) -> p k n", n=256)
            for half in range(2):
                ps, psb = self.bank()
                for kc in range(NKC):
                    self.pe(lambda e, ps=ps, sv=sv, half=half, kc=kc: e.matmul(
                        ps[:, :W], lhsT=sv[:, kc, half * 128:(half + 1) * 128], rhs=hT[:, kc, :W],
                        start=(kc == 0), stop=(kc == NKC - 1)), r=[sbuf_, self.hb[kc]], w=[psb])
                hh = (i % 4) * 2 + half
                dst, dstb, scale = (qst, qb, DK ** -0.5) if i < 4 else (kst, kb, 1.0)
                self.act(lambda e, ps=ps, dst=dst, hh=hh, scale=scale: e.activation(
                    out=dst[:, 0:nsub, hh, :], in_=ps[:, :W].rearrange("p (s j) -> p s j", j=128),
                    func=AF.Copy, scale=scale), r=[psb], w=dstb)
    for i in range(10):
        if is_ctx and i >= 6:
            continue
        slot, sbuf_ = self.wload(dd["wtm"][i], NKC * 512, dd.get("c_wtm"), i)
        sv = slot[:, 0:NKC * 512].rearrange("p (k n) -> p k n", n=512)
        for sub in range(nsub):
            ps, psb = self.bank()
            for kc in range(NKC):
                self.pe(lambda e, ps=ps, sv=sv, sub=sub, kc=kc: e.matmul(
                    ps[:, :], lhsT=hT[:, kc, sub * 128:(sub + 1) * 128], rhs=sv[:, kc, :],
                    start=(kc == 0), stop=(kc == NKC - 1)), r=[sbuf_, self.hb[kc]], w=[psb])
            if i < 2:
                self.dve(lambda e, ps=ps, sub=sub, i=i: e.tensor_copy(out=ktm[:, sub, i * 512:(i + 1) * 512],
                                                                   in_=ps[:, :]), r=[psb], w=ktb)
            elif i < 6:
                vi = i - 2
                self.dve(lambda e, ps=ps, sub=sub, vi=vi: e.tensor_copy(
                    out=Vst[:, sub, 2 * vi:2 * vi + 2, 0:DV], in_=ps[:, :].rearrange("p (h e) -> p h e", e=DV)),
                    r=[psb], w=[Vb])
            else:
                oi = i - 6
                self.act(lambda e, ps=ps, sub=sub, oi=oi: e.activation(
                    out=osg[:, sub, oi * 512:(oi + 1) * 512], in_=ps[:, :], func=AF.Sigmoid), r=[psb], w=ob)
    slot, sbuf_ = self.wload(dd["wgate"], NKC * 128, dd.get("c_wgate"), 0)
    sv = slot[:, 0:NKC * 128].rearrange("p (k n) -> p k n", n=128)
    pi_, pib = self.bank()
    pf_, pfb = self.bank()
    for half, (ps, psb) in enumerate(((pi_, pib), (pf_, pfb))):
        for kc in range(NKC):
            self.pe(lambda e, ps=ps, sv=sv, half=half, kc=kc: e.matmul(
                ps[0:64, :W], lhsT=sv[:, kc, half * 64:(half + 1) * 64], rhs=hT[:, kc, :W],
                start=(kc == 0), stop=(kc == NKC - 1)), r=[sbuf_, self.hb[kc]], w=[psb])
    Li, Lf, gb = (self.Li_c, self.Lf_c, self.gcb) if kind == "ctx" else (self.Li_x, self.Lf_x, self.gxb)
    t0 = 0 if kind == "ctx" else c0 * 128
    self.act(lambda e: e.activation(out=Li[0:64, t0:t0 + W], in_=pi_[0:64, :W], func=AF.Identity,
                                    bias=self.bg[0:64, 0:1], scale=1.0), r=[pib, self.bgb], w=[gb])
    t, tb = self.gettmp()
    self.act(lambda e, t=t: e.activation(out=t[0:64, :W], in_=pf_[0:64, :W], func=AF.Exp,
                                         bias=self.bg[0:64, 2:3], scale=-1.0), r=[pfb, self.bgb], w=[tb])
    self.act(lambda e, t=t: e.activation(out=t[0:64, :W], in_=t[0:64, :W], func=AF.Ln,
                                         bias=self.one_t[0:64, :], scale=1.0), r=[tb, self.b_const], w=[tb])
    self.dve(lambda e, t=t: e.tensor_scalar(out=Lf[0:64, t0:t0 + W], in0=t[0:64, :W], scalar1=-1.0, scalar2=None,
                                            op0=ALU.mult), r=[tb], w=[gb])
    sfx = {"ctx": "c", "other": "o", "own": "x"}[kind]
    if not is_ctx:
        self.dma(dd["qT_s"][c0:c0 + nsub].rearrange("c p n -> p c n"), Rf[:, 0:nsub * 1024].rearrange("p (s n) -> p s n", s=nsub),
                 r=qb, w=[dd["b_qT"]], stream="st_q")
        self.dma(dd["kT_s"][c0:c0 + nsub].rearrange("c p n -> p c n"), Rf[:, 4096:4096 + nsub * 1024].rearrange("p (s n) -> p s n", s=nsub),
                 r=kb, w=[dd["b_kT"]], stream="st_k")
        self.dma(dd["osig_s"][c0:c0 + nsub].rearrange("c p n -> p c n"), osg[:, 0:nsub, :], r=ob, w=[dd["b_osig"]], stream="st_o")
    self.dma(dd["ktm_" + sfx][c0:c0 + nsub].rearrange("c p n -> p c n"), ktm[:, 0:nsub, :], r=ktb, w=[dd["b_ktm" + sfx]], stream="st_kt")
    self.dma(dd["V_" + sfx][c0:c0 + nsub].rearrange("c p n -> p c n"),
             Vst[:, 0:nsub].rearrange("p s h e -> p s (h e)"), r=[Vb], w=[dd["b_V" + sfx]], stream="st_v")


def _gate_prep(self, tag, Li, Lf, gb, NC, d, start_kind, out):
    rs = slice(0, 8) if d == 0 else slice(32, 40)
    A64 = slice(0, 64)
    g = self.gp
    bF, u, aE, cE = g["bF"], g["u"], g["aE"], g["cE"]
    tot, Am, M, mB, wI = g["tot"], g["A"], g["M"], g["mB"], g["wI"]
    b = g["b"]
    order = list(range(NC)) if d == 0 else list(range(NC - 1, -1, -1))
    v3 = lambda t: t[A64, 0:NC * 128].rearrange("p (c j) -> p c j", j=128)
    bF3, u3, aE3, cE3 = v3(bF), v3(u), v3(aE), v3(cE)
    Li3 = Li[A64, 0:NC * 128].rearrange("p (c j) -> p c j", j=128)
    Lf3 = Lf[A64, 0:NC * 128].rearrange("p (c j) -> p c j", j=128)
    for c in range(NC):
        self.dve(lambda e, c=c: e.tensor_tensor_scan(out=bF3[:, c, :], data0=self.ones_f[A64, :], data1=Lf3[:, c, :],
                                                     initial=0.0, op0=ALU.mult, op1=ALU.add),
                 r=[gb, self.b_const], w=[b])
    self.dve(lambda e: e.tensor_copy(out=tot[A64, 0:NC], in_=bF3[:, :, 127]), r=[b], w=[b])
    if d == 1:
        r1 = slice(32, 64)
        self.dve(lambda e: e.tensor_tensor(out=bF3[r1], in0=Lf3[r1], in1=bF3[r1], op=ALU.subtract), r=[b, gb], w=[b])
        self.dve(lambda e: e.tensor_tensor(out=bF3[r1], in0=bF3[r1],
                                           in1=tot[r1, 0:NC].unsqueeze(2).broadcast_to([32, NC, 128]), op=ALU.add),
                 r=[b], w=[b])
    self.dve(lambda e: e.tensor_tensor(out=u3, in0=Li3, in1=bF3, op=ALU.subtract), r=[b, gb], w=[b])
    self.dve(lambda e: e.tensor_reduce(out=Am[A64, 0:NC], in_=u3, axis=AX.X, op=ALU.max), r=[b], w=[b])
    for k, c in enumerate(order):
        if k == 0 and start_kind == "empty":
            self.dve(lambda e, c=c: e.tensor_copy(out=M[rs, c:c + 1], in_=Am[rs, c:c + 1]), r=[b], w=[b])
            self.dve(lambda e, c=c: e.tensor_copy(out=mB[rs, c:c + 1], in_=Am[rs, c:c + 1]), r=[b], w=[b])
        else:
            self.dve(lambda e, c=c: e.tensor_tensor(out=M[rs, c:c + 1], in0=mB[rs, c:c + 1], in1=Am[rs, c:c + 1],
                                                    op=ALU.max), r=[b], w=[b])
        dst = mB[rs, order[k + 1]:order[k + 1] + 1] if k + 1 < NC else g["mfin"][rs, 0:1]
        self.dve(lambda e, c=c, dst=dst: e.tensor_tensor(out=dst, in0=M[rs, c:c + 1], in1=tot[rs, c:c + 1], op=ALU.add),
                 r=[b], w=[b])
    self.dve(lambda e: e.tensor_tensor(out=wI[A64, 0:NC], in0=mB[A64, 0:NC], in1=M[A64, 0:NC], op=ALU.subtract), r=[b], w=[b])
    self.act(lambda e: e.activation(out=wI[A64, 0:NC], in_=wI[A64, 0:NC], func=AF.Exp), r=[b], w=[b])
    Mb = M[A64, 0:NC].unsqueeze(2).broadcast_to([64, NC, 128])
    self.dve(lambda e: e.tensor_tensor(out=aE3, in0=u3, in1=Mb, op=ALU.subtract), r=[b], w=[b])
    self.act(lambda e: e.activation(out=aE3, in_=aE3, func=AF.Exp), r=[b], w=[b])
    self.dve(lambda e: e.scalar_tensor_tensor(out=cE3, in0=bF3, scalar=-1.0, in1=Mb, op0=ALU.mult, op1=ALU.subtract),
             r=[b], w=[b])
    self.act(lambda e: e.activation(out=cE3, in_=cE3, func=AF.Exp), r=[b], w=[b])
    idn = self.cst[0:64, 256:320]
    for src3, dst in ((aE3, out["aT"]), (cE3, out["clT"])):
        for half in range((NC + 7) // 8):
            ps, psb = self.bank()
            n = min(8, NC - half * 8)
            for cc in range(n):
                c = half * 8 + cc
                self.pe(lambda e, ps=ps, cc=cc, c=c, src3=src3: e.matmul(
                    ps[:, cc * 64:(cc + 1) * 64], lhsT=src3[rs, c, :], rhs=idn[rs, :], start=True, stop=True),
                    r=[b, self.cstb], w=[psb])
            self.dve(lambda e, ps=ps, n=n, half=half, dst=dst: e.tensor_copy(
                out=dst[:, half * 8:half * 8 + n, :],
                in_=ps[:, 0:n * 64].rearrange("p (c r) -> p c r", r=64)[:, :, rs]), r=[psb], w=[out["b"]])
    for half in range((NC + 7) // 8):
        n = min(8, NC - half * 8)
        t, tb = self.gettmp()
        t3 = t[0:64, 0:n * 64].rearrange("p (c r) -> p c r", r=64)
        self.dve(lambda e, t3=t3, n=n, half=half: e.tensor_tensor(
            out=t3, in0=wI[A64, half * 8:half * 8 + n].unsqueeze(2).broadcast_to([64, n, 64]),
            in1=idn.unsqueeze(1).broadcast_to([64, n, 64]), op=ALU.mult), r=[b, self.cstb], w=[tb])
        ps, psb = self.bank()
        self.pe(lambda e, ps=ps, t=t, n=n: e.matmul(ps[:, 0:n * 64], lhsT=self.ones_f[rs, :], rhs=t[rs, 0:n * 64],
                                                    start=True, stop=True), r=[tb, self.b_const], w=[psb])
        self.dve(lambda e, ps=ps, n=n, half=half: e.tensor_copy(
            out=out["wB"][:, half * 8:half * 8 + n, :],
            in_=ps[:, 0:n * 64].rearrange("p (c r) -> p c r", r=64)[:, :, rs]), r=[psb], w=[out["b"]])


def _scan(self, NC, d, gp, dd, sfx, state_only, start_empty, accumulate):
    order = list(range(NC)) if d == 0 else list(range(NC - 1, -1, -1))
    s = self.sc
    C, Cbuf = self.C, self.Cb_
    C3 = C[:].rearrange("p (h e) -> p h e", e=DVE_)
    mask = self.cst[:, d * 128:(d + 1) * 128]
    def loads(k):
        c = order[k]
        i2 = k % 2
        lb = s["lb"][i2]
        if not state_only:
            self.dma(s["q"][i2][:], dd["qT_s"][c], r=[dd["b_qT"]], w=[lb[0]], stream=f"ld_q{i2}")
            self.dma(s["k"][i2][:], dd["kT_s"][c], r=[dd["b_kT"]], w=[lb[1]], stream=f"ld_k{i2}")
        self.dma(s["kt"][i2][:], dd["ktm_" + sfx][c], r=[dd["b_ktm" + sfx]], w=[lb[2]], stream=f"ld_kt{i2}")
        self.dma(s["V"][i2][:], dd["V_" + sfx][c], r=[dd["b_V" + sfx]], w=[lb[3]], stream=f"ld_v{i2}")
        if accumulate:
            self.dma(s["H1"][i2][:], dd["H_rd"][c], r=[dd["b_Hin"]], w=[s["H1b"][i2]], stream=f"ld_h{i2}")

    loads(0)
    for k, c in enumerate(order):
        i2 = k % 2
        if k + 1 < NC:
            loads(k + 1)
        qc, kc_, ktc, Vc = s["q"][i2], s["k"][i2], s["kt"][i2], s["V"][i2]
        lb = s["lb"][i2]
        if accumulate:
            H1 = s["H1"][i2]
        Vt, Vtb = s["Vt"], s["Vtb"]
        V3 = Vc[:].rearrange("p (h e) -> p h e", e=DVE_)
        Vt3 = Vt[:].rearrange("p (h e) -> p h e", e=DVE_)
        self.dve(lambda e, V3=V3, c=c: e.tensor_tensor(
            out=Vt3, in0=V3, in1=gp["aT"][:, c, :].unsqueeze(2).broadcast_to([128, 8, DVE_]), op=ALU.mult),
            r=[lb[3], gp["b"]], w=[Vtb])
        first = (k == 0 and start_empty)
        if not state_only and not first:
            Cbf, Cbfb = s["Cbf"], s["Cbfb"]
            Cbf3 = Cbf[:].rearrange("p (h e) -> p h e", e=DVE_)
            self.dve(lambda e, c=c: e.tensor_tensor(
                out=Cbf3, in0=C3, in1=gp["wB"][:, c, :].unsqueeze(2).broadcast_to([128, 8, DVE_]), op=ALU.mult),
                r=[Cbuf, gp["b"]], w=[Cbfb])
        if not state_only:
            Ho, Hob = s["Ho"][i2], s["Hob"][i2]
        LA = 2
        pSd = {}

        def emit_S(h):
            pS, pSb = self.bank()
            self.pe(lambda e, pS=pS, h=h, qc=qc, kc_=kc_: e.matmul(
                pS[:, 0:128], lhsT=kc_[:, h * 128:(h + 1) * 128], rhs=qc[:, h * 128:(h + 1) * 128],
                start=True, stop=True), r=[lb[0], lb[1]], w=[pSb])
            pSd[h] = (pS, pSb)

        def emit_mask(h):
            pS, pSb = pSd[h]
            St, Stb = s["St"][h % 4], s["Stb"][h % 4]
            self.dve(lambda e, pS=pS, St=St: e.tensor_tensor(out=St[:], in0=pS[:, 0:128], in1=mask, op=ALU.mult),
                     r=[pSb, self.cstb], w=[Stb])

        if not state_only:
            for h in range(min(LA, H)):
                emit_S(h)
                emit_mask(h)
        for h in range(H):
            if not state_only:
                if h + LA < H:
                    emit_S(h + LA)
                St, Stb = s["St"][h % 4], s["Stb"][h % 4]
                pH, pHb = self.bank()
                self.pe(lambda e, pH=pH, St=St, h=h, first=first: e.matmul(
                    pH[:, 0:DVE_], lhsT=St[:], rhs=Vt3[:, h, :], start=True, stop=first), r=[Stb, Vtb], w=[pHb])
                if not first:
                    self.pe(lambda e, pH=pH, h=h, qc=qc: e.matmul(
                        pH[:, 0:DVE_], lhsT=qc[:, h * 128:(h + 1) * 128], rhs=Cbf3[:, h, :], start=False, stop=True),
                        r=[lb[0], Cbfb], w=[pHb])
            pK, pKb = self.bank()
            self.pe(lambda e, pK=pK, h=h, ktc=ktc: e.matmul(
                pK[:, 0:DVE_], lhsT=ktc[:, h * 128:(h + 1) * 128], rhs=Vt3[:, h, :], start=True, stop=True),
                r=[lb[2], Vtb], w=[pKb])
            if not state_only:
                if h + LA < H:
                    emit_mask(h + LA)
                dn, dnb = s["dn"][h % 2], s["dnb"][h % 2]
                self.dve(lambda e, pH=pH, dn=dn: e.tensor_scalar(
                    out=dn[:, 1:2], in0=pH[:, DV:DVE_], scalar1=-1.0, scalar2=None, op0=ALU.mult),
                    r=[pHb], w=[dnb])
                self.dve(lambda e, pH=pH, dn=dn, c=c, h=h: e.scalar_tensor_tensor(
                    out=dn[:, 0:1], in0=pH[:, DV:DVE_], scalar=gp["clT"][:, c, h:h + 1], in1=dn[:, 1:2],
                    op0=ALU.max, op1=ALU.max), r=[pHb, gp["b"], dnb], w=[dnb])
                self.dve(lambda e, dn=dn: e.reciprocal(out=dn[:, 1:2], in_=dn[:, 0:1]), r=[dnb], w=[dnb])
                if accumulate:
                    self.dve(lambda e, pH=pH, dn=dn, h=h, Ho=Ho, H1=H1: e.scalar_tensor_tensor(
                        out=Ho[:, h * DV:(h + 1) * DV], in0=pH[:, 0:DV], scalar=dn[:, 1:2],
                        in1=H1[:, h * DV:(h + 1) * DV], op0=ALU.mult, op1=ALU.add),
                        r=[pHb, dnb, s["H1b"][i2]], w=[Hob])
                else:
                    self.act(lambda e, pH=pH, dn=dn, h=h, Ho=Ho: e.activation(
                        out=Ho[:, h * DV:(h + 1) * DV], in_=pH[:, 0:DV], func=AF.Copy, scale=dn[:, 1:2]),
                        r=[pHb, dnb], w=[Hob])
            if first:
                self.dve(lambda e, pK=pK, h=h: e.tensor_copy(out=C3[:, h, :], in_=pK[:, 0:DVE_]), r=[pKb], w=[Cbuf])
            else:
                self.dve(lambda e, pK=pK, h=h, c=c: e.scalar_tensor_tensor(
                    out=C3[:, h, :], in0=C3[:, h, :], scalar=gp["wB"][:, c, h:h + 1], in1=pK[:, 0:DVE_],
                    op0=ALU.mult, op1=ALU.add), r=[pKb, gp["b"], Cbuf], w=[Cbuf])
        if not state_only:
            self.dma(dd["H_wr"][c], Ho[:], r=[Hob], w=[dd["b_Hout"]], stream=f"st_h{i2}")
        if self.bg_gen is not None:
            if next(self.bg_gen, "done") == "done":
                self.bg_gen = None


Builder.barrier = _barrier
Builder.Rbufs = _Rbufs
Builder.mlstm_proj = _mlstm_proj
Builder.gate_prep = _gate_prep
Builder.scan = _scan


def _mixer_out(self, W, c0, G, mb, dd):
    nsub = W // 128
    xT, hT = self.xT, self.hT
    idb = self.identb
    for sub in range(nsub):
        c = c0 + sub
        i2 = 0
        Hc, Hcb = self.Hc[i2], self.Hcb[i2]
        og, ogb = self.og[i2], self.ogb[i2]
        self.dma(Hc[:], dd["H_fin"][c], r=[dd["b_Hout"]], w=[Hcb], stream=f"ld_H{i2}")
        self.dma(og[:], dd["osig_s"][c], r=[dd["b_osig"]], w=[ogb], stream=f"ld_og{i2}")
        sqf, sqfb = self.sqf, self.sqfb
        self.dve(lambda e, Hc=Hc: e.tensor_tensor(out=sqf[:], in0=Hc[:], in1=Hc[:], op=ALU.mult), r=[Hcb], w=[sqfb])
        ss, ssb = self.ss, self.ssb
        self.dve(lambda e: e.tensor_reduce(out=ss[:, 0:8], in_=sqf[:].rearrange("p (h e) -> p h e", e=DV),
                                           axis=AX.X, op=ALU.add), r=[sqfb], w=[ssb])
        self.act(lambda e: e.activation(out=ss[:, 8:16], in_=ss[:, 0:8], func=AF.Sqrt, bias=self.eps_t[:],
                                        scale=1.0 / DV), r=[ssb, self.b_const], w=[ssb])
        self.dve(lambda e: e.reciprocal(out=ss[:, 16:24], in_=ss[:, 8:16]), r=[ssb], w=[ssb])
        self.dve(lambda e, Hc=Hc: e.tensor_tensor(
            out=sqf[:].rearrange("p (h e) -> p h e", e=DV), in0=Hc[:].rearrange("p (h e) -> p h e", e=DV),
            in1=ss[:, 16:24].unsqueeze(2).broadcast_to([128, 8, DV]), op=ALU.mult), r=[Hcb, ssb, sqfb], w=[sqfb])
        self.dve(lambda e: e.tensor_tensor(out=sqf[:], in0=sqf[:], in1=self.gN[:], op=ALU.mult),
                 r=[sqfb, self.gNb], w=[sqfb])
        yb = self.R[:].rearrange("p c t -> p (c t)")[:, 0:D]
        ybl = self.Rb[0:4]
        self.dve(lambda e, og=og, yb=yb: e.tensor_tensor(out=yb, in0=sqf[:], in1=og[:], op=ALU.mult),
                 r=[sqfb, ogb], w=ybl)
        for g4 in range(4):
            ps, psb = self.bank()
            psv = ps[:].bitcast(BF16)
            for j in range(4):
                kc = g4 * 4 + j
                self.pe(lambda e, psv=psv, j=j, kc=kc, yb=yb: e.transpose(
                    out=psv[:, j * 128:(j + 1) * 128], in_=yb[:, kc * 128:(kc + 1) * 128], identity=idb[:]),
                    r=ybl + [self.cstb], w=[psb])
            self.act(lambda e, psv=psv, g4=g4, sub=sub: e.activation(
                out=hT[:, g4 * 4:g4 * 4 + 4, sub * 128:(sub + 1) * 128],
                in_=psv[:, 0:512].rearrange("p (k t) -> p k t", t=128), func=AF.Copy),
                r=[psb], w=self.hb[g4 * 4:g4 * 4 + 4])
    self.proj_out(W, dd["mwout"], G, mb, self.hT, self.hb, dd.get("c_mwout"))


def _proj_out(self, W, wtiles, G, mb, src, srcb, cache=None):
    xT = self.xT
    pend = None
    for i in range(8):
        slot, sbuf_ = self.wload(wtiles[i], NKC * 256, cache, i)
        sv = slot[:, 0:NKC * 256].rearrange("p (k n) -> p k n", n=256)
        for half in range(2):
            m = 2 * i + half
            ps, psb = self.bank()
            for kc in range(NKC):
                self.pe(lambda e, ps=ps, sv=sv, half=half, kc=kc: e.matmul(
                    ps[:, :W], lhsT=sv[:, kc, half * 128:(half + 1) * 128], rhs=src[:, kc, :W],
                    start=(kc == 0), stop=(kc == NKC - 1)), r=[sbuf_, srcb[kc]], w=[psb])
            if pend is not None:
                self.stat_accum(pend, W)
                pend = None
            self.dve(lambda e, ps=ps, m=m: e.scalar_tensor_tensor(
                out=xT[:, m, :W], in0=ps[:, :W], scalar=G[:, m:m + 1], in1=xT[:, m, :W],
                op0=ALU.mult, op1=ALU.add), r=[psb, mb, self.xb[m]], w=[self.xb[m]])
            pend = self.stat_square(m, W)
    self.stat_accum(pend, W)
    self.stats_ready = W


def _conv_mixer(self, W, mc, dd):
    A, Sh, G, mb = mc
    hT, R = self.hT, self.R
    cw = self.cw
    self.rms_stats(W)
    self.modulate(W, mc)
    nr = W // 64
    for i in range(NKC):
        slot, sbuf_ = self.wload(dd["cwin"][i], NKC * 384, dd.get("c_cwin"), i)
        sv = slot[:, 0:NKC * 384].rearrange("p (k n) -> p k n", n=384)
        pss = []
        for part in range(3):
            ps, psb = self.bank()
            pss.append((ps, psb))
            for kc in range(NKC):
                self.pe(lambda e, ps=ps, sv=sv, part=part, kc=kc: e.matmul(
                    ps[:, :W], lhsT=sv[:, kc, part * 128:(part + 1) * 128], rhs=hT[:, kc, :W],
                    start=(kc == 0), stop=(kc == NKC - 1)), r=[sbuf_, self.hb[kc]], w=[psb])
        (pbg, pbgb), (pcg, pcgb), (pu, pub) = pss
        t1, t1b = self.gettmp()
        self.act(lambda e, t1=t1, pcg=pcg: e.activation(out=t1[:, :W], in_=pcg[:, :W], func=AF.Copy), r=[pcgb], w=[t1b])
        z, zb = self.gettmp()
        self.dve(lambda e, z=z, t1=t1, pu=pu: e.tensor_tensor(out=z[:, :W], in0=t1[:, :W], in1=pu[:, :W], op=ALU.mult),
                 r=[t1b, pub], w=[zb])
        zc, zcb = self.gettmp()
        self.dve(lambda e, z=z, zc=zc, i=i: e.tensor_scalar(out=zc[:, :W], in0=z[:, :W], scalar1=cw[:, i, 1:2],
                                                          scalar2=None, op0=ALU.mult), r=[zb, self.cwb], w=[zcb])
        z3 = z[:, :W].rearrange("p (r j) -> p r j", j=64)
        zc3 = zc[:, :W].rearrange("p (r j) -> p r j", j=64)
        self.dve(lambda e, z3=z3, zc3=zc3, i=i: e.scalar_tensor_tensor(
            out=zc3[:, :, 1:64], in0=z3[:, :, 0:63], scalar=cw[:, i, 0:1], in1=zc3[:, :, 1:64],
            op0=ALU.mult, op1=ALU.add), r=[zb, zcb, self.cwb], w=[zcb])
        self.dve(lambda e, z3=z3, zc3=zc3, i=i: e.scalar_tensor_tensor(
            out=zc3[:, :, 0:63], in0=z3[:, :, 1:64], scalar=cw[:, i, 2:3], in1=zc3[:, :, 0:63],
            op0=ALU.mult, op1=ALU.add), r=[zb, zcb, self.cwb], w=[zcb])
        self.dve(lambda e, zc=zc, pbg=pbg, i=i: e.tensor_tensor(out=R[:, i, :W], in0=pbg[:, :W], in1=zc[:, :W],
                                                              op=ALU.mult), r=[pbgb, zcb], w=[self.Rb[i]])
    self.proj_out(W, dd["cwout"], G, mb, R, self.Rb, dd.get("c_cwout"))


def _final_norm(self, W, fg, fgb):
    xT = self.xT
    rstd = self.rstd
    self.rms_stats(W)
    for c in range(NKC):
        self.dve(lambda e, c=c: e.scalar_tensor_tensor(
            out=xT[:, c, :W], in0=xT[:, c, :W], scalar=fg[:, c:c + 1], in1=rstd[:, :W],
            op0=ALU.mult, op1=ALU.mult), r=[self.xb[c], fgb, self.rstdb], w=[self.xb[c]])


Builder.mixer_out = _mixer_out
Builder.proj_out = _proj_out
Builder.conv_mixer = _conv_mixer
Builder.final_norm = _final_norm


def build_program(mode):
    b = Builder(mode)
    nc = b.nc
    dd = {}
    A_ = mode in "AF"
    B_ = mode in "BF"
    cT_d = b.din("cT", [128, NKC, 2])
    normg_d = b.din("normgT", [128, 96])
    cst_d = b.din("consts", [128, 384])
    if A_:
        xT_d = b.din("xT", [NKC, 128, TOK])
        ctxT_d = b.din("ctxT", [NKC, 128, CTX])
        wmod0 = [b.din("wmod0a", [18, 128, NKC * 512]), b.din("wmod0b", [18, 128, NKC * 512])]
        bmod0 = b.din("bmodT0", [128, 144])
        fin = {0: b.din("ffn_in0", [NFC, 128, NKC * 256])}
        fout = {0: b.din("ffn_out0", [NKC, 128, NFC * 128])}
        dd["wqk"] = b.din("wqk", [8, 128, NKC * 256])
        dd["wtm"] = b.din("wtm", [10, 128, NKC * 512])
        dd["wgate"] = b.din("wgate", [128, NKC * 128])
        bg_d = b.din("bgate", [64, 2])
    if B_:
        wmod1 = [b.din("wmod1a", [18, 128, NKC * 512]), b.din("wmod1b", [18, 128, NKC * 512])]
        bmod1 = b.din("bmodT1", [128, 144])
        if not A_:
            fin, fout = {}, {}
        for i in (1, 2, 3):
            fin[i] = b.din(f"ffn_in{i}", [NFC, 128, NKC * 256])
            fout[i] = b.din(f"ffn_out{i}", [NKC, 128, NFC * 128])
        dd["mwout"] = b.din("mwout", [8, 128, NKC * 256])
        mng_d = b.din("mnormg", [128, D])
        dd["cwin"] = b.din("cwin", [NKC, 128, NKC * 384])
        cwT_d = b.din("cwT", [128, 48])
        dd["cwout"] = b.din("cwout", [8, 128, NKC * 256])
        fng_d = b.din("fnormgT", [128, NKC])
        out_d = b.dout("outT", [NKC, 128, TOK])
    xs = b.scratch("xs", [NKC, 128, TOK], F32, "A")
    dd["qT_s"] = b.scratch("qT_s", [NCH, 128, 1024], BF16, "A")
    dd["kT_s"] = b.scratch("kT_s", [NCH, 128, 1024], BF16, "A")
    dd["ktm_x"] = b.scratch("ktm_x", [NCH, 128, 1024], BF16, "A")
    dd["V_x"] = b.scratch("V_x", [NCH, 128, H * DVE_], BF16, "A")
    dd["osig_s"] = b.scratch("osig_s", [NCH, 128, D], BF16, "A")
    H1_s = b.scratch("H1_s", [NCH, 128, D], F32, "A")
    Li_s = b.scratch("Li_s", [64, TOK], F32, "A")
    Lf_s = b.scratch("Lf_s", [64, TOK], F32, "A")
    modT0_s = b.scratch("modT0_s", [128, 144, 2], F32, "A")
    if mode == "A":
        Cst_o = b.dout("Cst_o", [128, H * DVE_])
        mfin_o = b.dout("mfin_o", [64, 1])
    if mode == "B":
        C_in = b.din("C_in", [128, H * DVE_])
        m_in = b.din("m_in", [64, 1])
    c_fin0 = c_fout0 = None
    c_fin = {1: None, 2: None, 3: None}
    c_fout = {1: None, 2: None, 3: None}
    if mode == "F" and WCACHE:
        c_fin0 = b.mk_cache("c_fin0", NFC, NKC * 256)
        c_fout0 = b.mk_cache("c_fout0", NKC, NFC * 128)
        dd["c_wqk"] = b.mk_cache("c_wqk", 8, NKC * 256)
        dd["c_wtm"] = b.mk_cache("c_wtm", 10, NKC * 512)
        dd["c_wgate"] = b.mk_cache("c_wgate", 1, NKC * 128)
        dd["c_mwout"] = b.mk_cache("c_mwout", 8, NKC * 256)
        dd["c_cwin"] = b.mk_cache("c_cwin", NKC, NKC * 384)
        dd["c_cwout"] = b.mk_cache("c_cwout", 8, NKC * 256)
        c_fin = {i: b.mk_cache(f"c_fin{i}", NFC, NKC * 256) for i in (1, 2, 3)}
        c_fout = {i: b.mk_cache(f"c_fout{i}", NKC, NFC * 128) for i in (1, 2, 3)}
    if mode == "F" and USE_CC:
        sel_d = b.din("sel", [128, 2])
        cc_src = b.dram("cc_src", [128, H * DVE_ + 8], F32, "Internal")
        cc_dst = b.dram("cc_dst", [256, H * DVE_ + 8], F32, "Internal")
    if mode == "F" and not USE_CC:
        xT2_d = b.din("xT2", [NKC, 128, TOK])
        dd["ktm_o"] = b.dram("ktm_o", [NCH, 128, 1024], BF16, "Internal")
        dd["V_o"] = b.dram("V_o", [NCH, 128, H * DVE_], BF16, "Internal")
        Li_o = b.dram("Li_o", [64, TOK], F32, "Internal")
        Lf_o = b.dram("Lf_o", [64, TOK], F32, "Internal")
    if A_:
        dd["ktm_c"] = b.dram("ktm_c", [2, 128, 1024], BF16, "Internal")
        dd["V_c"] = b.dram("V_c", [2, 128, H * DVE_], BF16, "Internal")
    if B_:
        H2_s = b.dram("H2_s", [NCH, 128, D], F32, "Internal")
    for nm in ("qT", "kT", "ktmx", "Vx", "ktmc", "Vc", "ktmo", "Vo", "gates_o", "osig", "H1", "H2", "xs", "gates_s", "modT0_s", "state_o", "out"):
        dd["b_" + nm] = b.B("d_" + nm)

    b.setup_common()
    b.cst = b.sb("cst", [128, 384], F32)
    b.cstb = b.B("cst")
    b.dma(b.cst[:], cst_d, w=[b.cstb], stream="cst")
    b.identb = b.sb("identb", [128, 128], BF16)
    b.dve(lambda e: e.tensor_copy(out=b.identb[:], in_=b.cst[:, 256:384]), r=[b.cstb], w=[b.cstb])
    ng = b.sb("normg", [128, 96], F32)
    ngb = b.B("ng")
    b.dma(ng[:], normg_d, w=[ngb], stream="ng")
    mls = ExitStack()
    g = {}
    gpo = {}
    modT0 = b.sb("modT0", [128, 144, 2], F32)
    mod0b = b.B("modT0")

    def alloc_mls():
        b.cur = mls
        b.Li_x = b.sb("Li_x", [64, TOK], F32)
        b.Lf_x = b.sb("Lf_x", [64, TOK], F32)
        b.gxb = b.B("gx")
        b.C = b.sb("Cstate", [128, H * DVE_], F32)
        b.Cb_ = b.B("C")
        for nm in ("aT", "clT", "wB"):
            gpo[nm] = b.sb("gpo_" + nm, [128, NCH, 8], F32)
        gpo["b"] = b.B("gpo")
        g["b"] = b.B("g")
        for nm in ("tot", "A", "M", "mB", "wI"):
            g[nm] = b.sb("g_" + nm, [64, NCH], F32)
            b.dve(lambda e, t=g[nm]: e.memset(t[:], 0.0), w=[g["b"]])
        g["mfin"] = b.sb("g_mfin", [64, 1], F32)
        b.dve(lambda e: e.memset(g["mfin"][:], 0.0), w=[g["b"]])
        b.gp = g
        b.cur = None


    def alloc_scan():
        for nm in ("bF", "u", "aE", "cE"):
            g[nm] = b.sb("g_" + nm, [64, TOK], F32)
        s = {"q": [], "k": [], "kt": [], "V": [], "lb": [], "Ho": [], "Hob": [], "H1": [], "H1b": [],
             "St": [], "Stb": [], "dn": [], "dnb": []}
        for i in range(2):
            s["q"].append(b.sb(f"s_q{i}", [128, 1024], BF16))
            s["k"].append(b.sb(f"s_k{i}", [128, 1024], BF16))
            s["kt"].append(b.sb(f"s_kt{i}", [128, 1024], BF16))
            s["V"].append(b.sb(f"s_V{i}", [128, H * DVE_], BF16))
            s["lb"].append([b.B(f"lb{i}_{j}") for j in range(4)])
            s["Ho"].append(b.sb(f"s_Ho{i}", [128, D], F32))
            s["Hob"].append(b.B(f"Ho{i}"))
            if B_:
                s["H1"].append(b.sb(f"s_H1{i}", [128, D], F32))
                s["H1b"].append(b.B(f"H1{i}"))
            for j in range(2):
                s["St"].append(b.sb(f"s_St{i}_{j}", [128, 128], BF16))
                s["Stb"].append(b.B(f"St{i}_{j}"))
            s["dn"].append(b.sb(f"s_dn{i}", [128, 2], F32))
            s["dnb"].append(b.B(f"dn{i}"))
        s["Vt"] = b.sb("s_Vt", [128, H * DVE_], BF16)
        s["Vtb"] = b.B("Vt")
        s["Cbf"] = b.sb("s_Cbf", [128, H * DVE_], BF16)
        s["Cbfb"] = b.B("Cbf")
        b.sc = s
        b.tmp = [b.sb(f"stmp{i}", [128, NT], F32) for i in range(3)]
        b.tmpb = [b.B(f"stmp{i}") for i in range(3)]
        b.ti = 0

    if A_:
        b.bg = b.sb("bg", [64, 3], F32)
        b.bgb = b.B("bg")
        b.dma(b.bg[:, 0:2], bg_d, w=[b.bgb], stream="bg")
        b.dve(lambda e: e.tensor_scalar(out=b.bg[:, 2:3], in0=b.bg[:, 1:2], scalar1=-1.0, scalar2=None, op0=ALU.mult),
              r=[b.bgb], w=[b.bgb])
        b.Li_c = b.sb("Li_c", [64, CTX], F32)
        b.Lf_c = b.sb("Lf_c", [64, CTX], F32)
        b.gcb = b.B("gc")
        b.mod_phase(0, wmod0, bmod0, cT_d, modT0, mod0b)
        mcx0 = b.mod_consts(0, modT0, mod0b, ng, ngb, 0, "x")
        mcc0 = b.mod_consts(0, modT0, mod0b, ng, ngb, 1, "c")
        if mode == "F":
            modT1 = b.sb("modT1", [128, 144, 2], F32)
            mod1b = b.B("modT1")
            mod1_tl = b.mod_alloc(1)
            mc1_tiles = b.mod_consts_alloc(1, "x")
        alloc_mls()
        with ExitStack() as ph:
            b.cur = ph
            b.alloc_tile_state()
            b.Vst = b.sb("Vst", [128, 4, H, DVE_], BF16)
            b.Vstb = b.B("Vst")
            b.dve(lambda e: e.memset(b.Vst[:, :, :, DV:DVE_], 1.0), w=[b.Vstb])
            b.load_xT(ctxT_d, CTX)
            b.ffn(CTX, mcc0[0], fin[0], fout[0], c_in=c_fin0, c_out=c_fout0)
            ddc = dict(dd)
            ddc["b_ktmc"], ddc["b_Vc"] = dd["b_ktmc"], dd["b_Vc"]
            if STOP_AT >= 0:
                b.mlstm_proj(CTX, mcc0[1], 0, dd, "ctx")
            if mode == "F" and not USE_CC:
                for t in range(TOK // NT):
                    b.load_xT(xT2_d[:, :, t * NT:(t + 1) * NT], NT)
                    b.ffn(NT, mcx0[0], fin[0], fout[0])
                    b.mlstm_proj(NT, mcx0[1], t * 4, dd, "other")
                b.dma(Li_o, b.Li_x[:], r=[b.gxb], w=[dd["b_gates_o"]], stream="st_lio")
                b.dma(Lf_o, b.Lf_x[:], r=[b.gxb], w=[dd["b_gates_o"]], stream="st_lfo")
            for t in range(TOK // NT):
                b.load_xT(xT_d[:, :, t * NT:(t + 1) * NT], NT)
                b.ffn(NT, mcx0[0], fin[0], fout[0], c_in=c_fin0, c_out=c_fout0)
                b.store_xT(xs[:, :, t * NT:(t + 1) * NT], NT, wb=[dd["b_xs"]])
                if STOP_AT >= 0:
                    b.mlstm_proj(NT, mcx0[1], t * 4, dd, "own")
            b.barrier()
        with ExitStack() as ph:
            b.cur = ph
            alloc_scan()
            if mode == "F":
                b.bg_gen = b.mod_phase_gen(1, wmod1, bmod1, cT_d, modT1, mod1b, mod1_tl)
            if STOP_AT >= 1:
                b.gate_prep("c", b.Li_c, b.Lf_c, b.gcb, 2, 0, "empty", gpo)
            if STOP_AT >= 2:
                b.scan(2, 0, gpo, dd, "c", True, True, False)
            b.dve(lambda e: e.tensor_copy(out=g["mB"][0:8, 0:1], in_=g["mfin"][0:8, 0:1]), r=[g["b"]], w=[g["b"]])
            if STOP_AT >= 3:
                b.gate_prep("x", b.Li_x, b.Lf_x, b.gxb, NCH, 0, "state", gpo)
            dd1 = dict(dd)
            dd1["H_wr"], dd1["b_Hout"] = H1_s, dd["b_H1"]
            if STOP_AT >= 4:
                b.scan(NCH, 0, gpo, dd1, "x", False, False, False)
            if mode == "F" and USE_CC:
                NS = H * DVE_
                sel = b.sb("sel", [128, 2], F32)
                selb = b.B("sel")
                b.dma(sel[:], sel_d, w=[selb], stream="ld_sel")
                zt = b.sb("zt", [128, NS + 8], F32)
                ztb = b.B("zt")
                b.dve(lambda e: e.memset(zt[:], 0.0), w=[ztb])
                srcb, dstb = b.B("cc_src"), b.B("cc_dst")
                b.dma(cc_src, zt[:], r=[ztb], w=[srcb], stream="cc_z")
                b.dma(cc_src[:, 0:NS], b.C[:], r=[b.Cb_], w=[srcb], stream="cc_c")
                b.dma(cc_src[0:8, NS:NS + 1], g["mfin"][0:8, 0:1], r=[g["b"]], w=[srcb], stream="cc_m", slow=True)
                b.P.add("pool", lambda e: e.collective_compute(
                    "AllGather", ALU.bypass, replica_groups=[[0, 1], [2, 3], [4, 5], [6, 7]],
                    ins=[cc_src.opt()], outs=[cc_dst.opt()]), [srcb], [dstb], dma="cc", inc=1)
                for r in range(2):
                    b.dma(zt[:], cc_dst[r * 128:(r + 1) * 128, :], r=[dstb], w=[ztb], stream="cc_ld")
                    if r == 0:
                        b.dve(lambda e: e.tensor_scalar(out=b.C[:], in0=zt[:, 0:NS], scalar1=sel[:, 0:1], scalar2=None,
                                                        op0=ALU.mult), r=[ztb, selb], w=[b.Cb_])
                    else:
                        b.dve(lambda e: e.scalar_tensor_tensor(out=b.C[:], in0=zt[:, 0:NS], scalar=sel[:, 1:2],
                                                               in1=b.C[:], op0=ALU.mult, op1=ALU.add),
                              r=[ztb, selb, b.Cb_], w=[b.Cb_])
                mt = b.sb("mt", [64, 2], F32)
                mtb = b.B("mt")
                for r in range(2):
                    b.dma(mt[32:40, r:r + 1], cc_dst[r * 128:r * 128 + 8, NS:NS + 1], r=[dstb], w=[mtb], stream=f"cc_lm{r}", slow=True)
                b.dve(lambda e: e.tensor_tensor(out=mt[32:40, :], in0=mt[32:40, :], in1=sel[32:40, :], op=ALU.mult),
                      r=[mtb, selb], w=[mtb])
                b.dve(lambda e: e.tensor_tensor(out=g["mB"][32:40, NCH - 1:NCH], in0=mt[32:40, 0:1], in1=mt[32:40, 1:2],
                                                op=ALU.add), r=[mtb, g["b"]], w=[g["b"]])
                b.gate_prep("x2", b.Li_x, b.Lf_x, b.gxb, NCH, 1, "state", gpo)
                dd2 = dict(dd)
                dd2["H_rd"], dd2["b_Hin"] = H1_s, dd["b_H1"]
                dd2["H_wr"], dd2["b_Hout"] = H2_s, dd["b_H2"]
                b.scan(NCH, 1, gpo, dd2, "x", False, False, True)
            if mode == "F" and not USE_CC:
                b.gate_prep("c1", b.Li_c, b.Lf_c, b.gcb, 2, 1, "empty", gpo)
                b.scan(2, 1, gpo, dd, "c", True, True, False)
                b.dve(lambda e: e.tensor_copy(out=g["mB"][32:40, NCH - 1:NCH], in_=g["mfin"][32:40, 0:1]),
                      r=[g["b"]], w=[g["b"]])
                Li_t = b.sb("Li_t", [64, TOK], F32)
                Lf_t = b.sb("Lf_t", [64, TOK], F32)
                gtb = b.B("gt")
                b.dma(Li_t[:], Li_o, r=[dd["b_gates_o"]], w=[gtb], stream="ld_lio")
                b.dma(Lf_t[:], Lf_o, r=[dd["b_gates_o"]], w=[gtb], stream="ld_lfo")
                b.gate_prep("o1", Li_t, Lf_t, gtb, NCH, 1, "state", gpo)
                b.scan(NCH, 1, gpo, dd, "o", True, False, False)
                b.dve(lambda e: e.tensor_copy(out=g["mB"][32:40, NCH - 1:NCH], in_=g["mfin"][32:40, 0:1]),
                      r=[g["b"]], w=[g["b"]])
                b.gate_prep("x2", b.Li_x, b.Lf_x, b.gxb, NCH, 1, "state", gpo)
                dd2 = dict(dd)
                dd2["H_rd"], dd2["b_Hin"] = H1_s, dd["b_H1"]
                dd2["H_wr"], dd2["b_Hout"] = H2_s, dd["b_H2"]
                b.scan(NCH, 1, gpo, dd2, "x", False, False, True)
            if mode == "F":
                while b.bg_gen is not None:
                    if next(b.bg_gen, "done") == "done":
                        b.bg_gen = None
                mcx1 = b.mod_consts(1, modT1, mod1b, ng, ngb, 0, "x", tiles=mc1_tiles)
            if mode == "A":
                b.dma(Cst_o, b.C[:], r=[b.Cb_], w=[dd["b_state_o"]], stream="st_C")
                b.dma(mfin_o, g["mfin"][:], r=[g["b"]], w=[dd["b_out"]], stream="st_m")
                b.dma(Li_s, b.Li_x[:], r=[b.gxb], w=[dd["b_gates_s"]], stream="st_li")
                b.dma(Lf_s, b.Lf_x[:], r=[b.gxb], w=[dd["b_modT0_s"]], stream="st_lf")
                b.dma(modT0_s, modT0[:], r=[mod0b], w=[dd["b_xs"]], stream="st_mod")
            b.barrier()
        b.cur = None
    if mode == "A":
        mls.close()
        return b.finish([dd[k] for k in dd if k.startswith("b_")])
    if mode == "B":
        b.dma(modT0[:], modT0_s, w=[mod0b], stream="ld_mod")
        mcx0 = b.mod_consts(0, modT0, mod0b, ng, ngb, 0, "x")
    if mode == "B":
        modT1 = b.sb("modT1", [128, 144, 2], F32)
        mod1b = b.B("modT1")
        b.mod_phase(1, wmod1, bmod1, cT_d, modT1, mod1b)
        mcx1 = b.mod_consts(1, modT1, mod1b, ng, ngb, 0, "x")
    if mode == "B":
        alloc_mls()
        b.dma(b.Li_x[:], Li_s, w=[b.gxb], stream="ld_li")
        b.dma(b.Lf_x[:], Lf_s, w=[b.gxb], stream="ld_lf")
        b.dma(b.C[:], C_in, w=[b.Cb_], stream="ld_C")
        b.dma(g["mB"][32:40, NCH - 1:NCH], m_in[32:40, :], w=[g["b"]], stream="ld_m")
    if mode == "B":
        with ExitStack() as ph:
            b.cur = ph
            alloc_scan()
            b.gate_prep("x2", b.Li_x, b.Lf_x, b.gxb, NCH, 1, "state", gpo)
            dd2 = dict(dd)
            dd2["H_rd"], dd2["b_Hin"] = H1_s, dd["b_H1"]
            dd2["H_wr"], dd2["b_Hout"] = H2_s, dd["b_H2"]
            b.scan(NCH, 1, gpo, dd2, "x", False, False, True)
            b.barrier()
    mls.close()
    with ExitStack() as ph:
        b.cur = ph
        b.alloc_tile_state()
        b.Hc = [b.sb(f"Hc{i}", [128, D], F32) for i in range(1)]
        b.Hcb = [b.B(f"Hc{i}") for i in range(1)]
        b.og = [b.sb(f"og{i}", [128, D], BF16) for i in range(1)]
        b.ogb = [b.B(f"og{i}") for i in range(1)]
        b.sqf = b.sb("sqf", [128, D], F32)
        b.sqfb = b.B("sqf")
        b.ss = b.sb("ss", [128, 24], F32)
        b.ssb = b.B("ss")
        b.gN = b.sb("gN", [128, D], F32)
        b.gNb = b.B("gN")
        b.dma(b.gN[:], mng_d, w=[b.gNb], stream="ld_gN")
        cw = b.sb("cw", [128, 48], F32)
        b.cw = cw[:].rearrange("p (c k) -> p c k", k=3)
        b.cwb = b.B("cw")
        b.dma(cw[:], cwT_d, w=[b.cwb], stream="ld_cw")
        fg = b.sb("fg", [128, NKC], F32)
        fgb = b.B("fg")
        b.dma(fg[:], fng_d, w=[fgb], stream="ld_fg")
        dd3 = dict(dd)
        dd3["H_fin"], dd3["b_Hout"] = H2_s, dd["b_H2"]
        for t in range(TOK // NT):
            b.load_xT(xs[:, :, t * NT:(t + 1) * NT], NT)
            b.mixer_out(NT, t * 4, mcx0[1][2], mcx0[1][3], dd3)
            b.ffn(NT, mcx0[2], fin[1], fout[1], c_in=c_fin[1], c_out=c_fout[1])
            b.ffn(NT, mcx1[0], fin[2], fout[2], c_in=c_fin[2], c_out=c_fout[2])
            b.conv_mixer(NT, mcx1[1], dd3)
            b.ffn(NT, mcx1[2], fin[3], fout[3], c_in=c_fin[3], c_out=c_fout[3])
            b.final_norm(NT, fg, fgb)
            b.store_xT(out_d[:, :, t * NT:(t + 1) * NT], NT, wb=[dd["b_out"]])
    b.cur = None
    return b.finish([dd["b_out"]])


def _consts():
    j = np.arange(128)[:, None]
    s_ = np.arange(128)[None, :]
    c = np.zeros((128, 384), np.float32)
    c[:, 0:128] = (j <= s_)
    c[:, 128:256] = (j >= s_)
    c[:, 256:384] = np.eye(128, dtype=np.float32)
    return c


def _prep_mlstm(inp, flipped):
    w = inp["mlstm_w_in"][0]
    bgv = inp["mlstm_b_gate"][0]
    o2 = 2 * H * DK + H * DV
    wqk = _tiles_cols(w, [np.arange(i * 256, (i + 1) * 256) for i in range(8)])
    cols = [np.arange(1024 + i * 512, 1024 + (i + 1) * 512) for i in range(2)]
    cols += [np.arange(2048 + i * 512, 2048 + (i + 1) * 512) for i in range(4)]
    cols += [np.arange(o2 + 32 + i * 512, o2 + 32 + (i + 1) * 512) for i in range(4)]
    wtm = _tiles_cols(w, cols)
    gi = [0, 2] if not flipped else [2, 0]
    gf = [1, 3] if not flipped else [3, 1]
    wg = np.zeros((D, 128), np.float32)
    bg = np.zeros((64, 2), np.float32)
    for dirn in range(2):
        r0 = 32 * dirn
        wg[:, r0:r0 + 8] = w[:, o2 + gi[dirn] * 8:o2 + gi[dirn] * 8 + 8]
        wg[:, 64 + r0:64 + r0 + 8] = w[:, o2 + gf[dirn] * 8:o2 + gf[dirn] * 8 + 8]
        bg[r0:r0 + 8, 0] = bgv[gi[dirn] * 8:gi[dirn] * 8 + 8]
        bg[r0:r0 + 8, 1] = bgv[gf[dirn] * 8:gf[dirn] * 8 + 8]
    wgate = _tiles_cols(wg, [np.arange(128)])[0]
    return wqk, wtm, wgate, bg


_CACHE = {}


def kernel(x, c, ctx, c_ctx, w_mod, b_mod, norm_g, ffn_w_in, ffn_w_out,
           mlstm_w_in, mlstm_b_gate, mlstm_norm_g, mlstm_w_out,
           conv_w_in, conv_w, conv_w_out, final_norm_g):
    inp = dict(x=x, c=c, ctx=ctx, c_ctx=c_ctx, w_mod=w_mod, b_mod=b_mod, norm_g=norm_g, ffn_w_in=ffn_w_in,
               ffn_w_out=ffn_w_out, mlstm_w_in=mlstm_w_in, mlstm_b_gate=mlstm_b_gate, mlstm_norm_g=mlstm_norm_g,
               mlstm_w_out=mlstm_w_out, conv_w_in=conv_w_in, conv_w=conv_w, conv_w_out=conv_w_out,
               final_norm_g=final_norm_g)
    inp = {k: np.asarray(v, dtype=np.float32) for k, v in inp.items()}
    sh = prep_shared(inp)
    cst = _consts()
    ml = [_prep_mlstm(inp, False), _prep_mlstm(inp, True)]
    mwout = _tiles_cols(inp["mlstm_w_out"][0], [np.arange(i * 256, (i + 1) * 256) for i in range(8)])
    mnormg = np.ascontiguousarray(np.broadcast_to(inp["mlstm_norm_g"][0][None, :], (128, D)))
    cwin = _tiles_cols(inp["conv_w_in"][0], [np.concatenate([p * D + np.arange(i * 128, (i + 1) * 128) for p in range(3)])
                                             for i in range(NKC)])
    cwout = _tiles_cols(inp["conv_w_out"][0], [np.arange(i * 256, (i + 1) * 256) for i in range(8)])
    cwT = []
    for fl in range(2):
        cwv = inp["conv_w"][0][::-1] if fl else inp["conv_w"][0]
        cwT.append(np.ascontiguousarray(np.stack([_vecT(cwv[k]) for k in range(3)], axis=-1)).reshape(128, 48))
    n = 8
    mapsA, mapsB = [], []
    for k in range(n):
        bi, s = k // 2, k % 2
        xs_ = inp["x"][bi, s * TOK:(s + 1) * TOK]
        cx = inp["ctx"][bi]
        if s:
            xs_ = xs_[::-1]
            cx = cx[::-1]
        xT = np.ascontiguousarray(xs_.T).reshape(NKC, 128, TOK)
        ctxT = np.ascontiguousarray(cx.T).reshape(NKC, 128, CTX)
        cT = np.ascontiguousarray(np.stack([_vecT(inp["c"][bi]), _vecT(inp["c_ctx"])], axis=-1))
        wqk, wtm, wgate, bg = ml[s]
        mapsA.append({"cT": cT, "normgT": sh["normgT"], "consts": cst, "xT": xT, "ctxT": ctxT,
                      "wmod0a": sh["wmod"][0][:18], "wmod0b": sh["wmod"][0][18:], "bmodT0": sh["bmodT"][0],
                      "ffn_in0": sh["ffn_in"][0], "ffn_out0": sh["ffn_out"][0],
                      "wqk": wqk, "wtm": wtm, "wgate": wgate, "bgate": bg})
        mB = {"cT": cT, "normgT": sh["normgT"], "consts": cst,
              "wmod1a": sh["wmod"][1][:18], "wmod1b": sh["wmod"][1][18:], "bmodT1": sh["bmodT"][1],
              "mwout": mwout, "mnormg": mnormg, "cwin": cwin, "cwT": cwT[s], "cwout": cwout,
              "fnormgT": sh["fnormgT"]}
        for i in (1, 2, 3):
            mB[f"ffn_in{i}"] = sh["ffn_in"][i]
            mB[f"ffn_out{i}"] = sh["ffn_out"][i]
        mapsB.append(mB)
    if FUSED:
        mapsF = []
        for k in range(n):
            bi, s = k // 2, k % 2
            xo = inp["x"][bi, (1 - s) * TOK:(2 - s) * TOK]
            if s:
                xo = xo[::-1]
            m = dict(mapsA[k])
            m.update(mapsB[k])
            if USE_CC:
                selv = np.zeros((128, 2), np.float32)
                selv[:, 1 - s] = 1.0
                m["sel"] = selv
            else:
                m["xT2"] = np.ascontiguousarray(xo.T).reshape(NKC, 128, TOK)
            mapsF.append(m)
        if "F" not in _CACHE:
            _CACHE["F"] = build_program("F")
        resF = run_bass_kernel_spmd(_CACHE["F"], mapsF, core_ids=list(range(n))).results
        out = np.empty((4, 2 * TOK, D), np.float32)
        for k in range(n):
            bi, s = k // 2, k % 2
            o = resF[k]["outT"].reshape(D, TOK).T
            if s:
                o = o[::-1]
            out[bi, s * TOK:(s + 1) * TOK] = o
        return out
    if "A" not in _CACHE:
        _CACHE["A"] = build_program("A")
        _CACHE["B"] = build_program("B")
    resA = run_bass_kernel_spmd(_CACHE["A"], mapsA, core_ids=list(range(n))).results
    for k in range(n):
        p = k ^ 1
        for nm in ("xs", "qT_s", "kT_s", "ktm_x", "V_x", "osig_s", "H1_s", "Li_s", "Lf_s", "modT0_s"):
            mapsB[k][nm] = resA[k][nm]
        mapsB[k]["C_in"] = resA[p]["Cst_o"]
        m_in = np.zeros((64, 1), np.float32)
        m_in[32:40] = resA[p]["mfin_o"][0:8]
        mapsB[k]["m_in"] = m_in
    resB = run_bass_kernel_spmd(_CACHE["B"], mapsB, core_ids=list(range(n))).results
    out = np.empty((4, 2 * TOK, D), np.float32)
    for k in range(n):
        bi, s = k // 2, k % 2
        o = resB[k]["outT"].reshape(D, TOK).T
        if s:
            o = o[::-1]
        out[bi, s * TOK:(s + 1) * TOK] = o
    return out
```

```python
import numpy as np
from contextlib import ExitStack
import concourse.bass as bass
import concourse.mybir as mybir
from concourse.bass_utils import run_bass_kernel_spmd

F32 = mybir.dt.float32
BF16 = mybir.dt.bfloat16
ALU = mybir.AluOpType
AF = mybir.ActivationFunctionType
AX = mybir.AxisListType

D = 2048
NKC = 16
DFF = 5504
NFC = 43
H = 8
DK = 128
DV = 256
DVE_ = 257
TOK = 2048
NT = 512
NCH = 16
CTX = 256
EPS = 1e-6
WSLOT = 8192
NWS = 3
STOP_AT = 99
FUSED = True
USE_CC = True
WCACHE = True


class Buf:
    __slots__ = ("name", "last_w", "readers")

    def __init__(self, name):
        self.name = name
        self.last_w = None
        self.readers = []


class Op:
    __slots__ = ("idx", "eng", "stream", "pos", "fn", "deps", "signal", "value", "is_dma", "inc")


class Prog:
    ENGS = ("pe", "dve", "act", "pool", "sp")

    def __init__(self, nc):
        self.nc = nc
        self.ops = []
        self.stream_ops = {}

    def add(self, eng, fn, reads=(), writes=(), dma=None, inc=None):
        op = Op()
        op.inc = inc if inc is not None else (16 if dma is not None else 1)
        op.idx = len(self.ops)
        op.eng = eng
        op.stream = ("dma:" + dma) if dma else eng
        op.fn = fn
        op.is_dma = dma is not None
        op.signal = op.is_dma
        op.value = 0
        deps = set()
        for b in reads:
            if b.last_w is not None:
                deps.add(b.last_w)
        for b in writes:
            if b.last_w is not None:
                deps.add(b.last_w)
            deps.update(b.readers)
        deps.discard(op.idx)
        if eng == "pe" and not op.is_dma:
            deps = {d for d in deps if self.ops[d].stream != "pe"}
        op.deps = deps
        lst = self.stream_ops.setdefault(op.stream, [])
        op.pos = len(lst)
        lst.append(op.idx)
        self.ops.append(op)
        wset = set(id(b) for b in writes)
        for b in reads:
            if id(b) in wset:
                continue
            b.readers = [r for r in b.readers if self.ops[r].stream != op.stream]
            b.readers.append(op.idx)
        for b in writes:
            b.last_w = op.idx
            b.readers = []
        return op

    def emit(self):
        nc = self.nc
        ops = self.ops
        seen = {e: {} for e in self.ENGS}
        waits = [[] for _ in ops]
        for op in ops:
            need = {}
            for d in op.deps:
                P = ops[d]
                if need.get(P.stream, -1) < P.pos:
                    need[P.stream] = P.pos
            sE = seen[op.eng]
            for S, pos in need.items():
                if sE.get(S, -1) >= pos:
                    continue
                sE[S] = pos
                prod = ops[self.stream_ops[S][pos]]
                prod.signal = True
                waits[op.idx].append(prod.idx)
        for S, lst in self.stream_ops.items():
            cnt = 0
            for i in lst:
                if ops[i].signal:
                    cnt += ops[i].inc
                    ops[i].value = cnt
        with ExitStack() as es:
            sems = {}
            for S in self.stream_ops:
                sems[S] = es.enter_context(nc.semaphore("s_" + S.replace(":", "_")))
            block = es.enter_context(nc.Block())

            def run(engkey):
                def body(eng):
                    for op in ops:
                        if op.eng != engkey:
                            continue
                        for w in waits[op.idx]:
                            P = ops[w]
                            eng.wait_ge(sems[P.stream], P.value)
                        if op.fn is None:
                            continue
                        ins = op.fn(eng)
                        if op.signal:
                            ins.then_inc(sems[op.stream], op.inc)
                return body

            block.tensor(run("pe"))
            block.vector(run("dve"))
            block.scalar(run("act"))
            block.gpsimd(run("pool"))
            block.sync(run("sp"))


class Builder:
    def __init__(self, mode, dbg=None):
        self.mode = mode
        self.dbg = dbg or {}
        self.nc = bass.Bass("TRN2", target_bir_lowering=False)
        self.P = Prog(self.nc)
        self.es = ExitStack()
        self.nbuf = 0
        self.cur = None

    def sb(self, name, shape, dt=F32):
        self.nbuf += 1
        return (self.cur or self.es).enter_context(self.nc.sbuf_tensor(f"sb{self.nbuf}_" + name, list(shape), dt))

    def dram(self, name, shape, dt, kind):
        return self.nc.dram_tensor(name, list(shape), dt, kind=kind).ap()

    def din(self, name, shape, dt=F32):
        return self.dram(name, shape, dt, "ExternalInput")

    def dout(self, name, shape, dt=F32):
        return self.dram(name, shape, dt, "ExternalOutput")

    def scratch(self, name, shape, dt, produced_in):
        if self.mode == "F":
            return self.dram(name, shape, dt, "Internal")
        if self.mode == produced_in:
            return self.dout(name, shape, dt)
        return self.din(name, shape, dt)

    def B(self, name):
        self.nbuf += 1
        return Buf(name)

    def pe(self, fn, r=(), w=()):
        return self.P.add("pe", fn, r, w)

    def dve(self, fn, r=(), w=()):
        return self.P.add("dve", fn, r, w)

    def act(self, fn, r=(), w=()):
        return self.P.add("act", fn, r, w)

    def dma(self, out, in_, r=(), w=(), q="sp", stream=None, slow=False):
        assert stream is not None
        if slow:
            return self.P.add(q, lambda e: e.dma_start(out=out, in_=in_, allow_slow_non_contiguous=True), r, w, dma=stream)
        return self.P.add(q, lambda e: e.dma_start(out=out, in_=in_), r, w, dma=stream)

    def setup_common(self):
        nc = self.nc
        self.wslots = [self.sb(f"wslot{i}", [128, WSLOT], BF16) for i in range(NWS)]
        self.wbufs = [self.B(f"wslot{i}") for i in range(NWS)]
        self.wi = 0
        self.psum = [self.es.enter_context(nc.psum_tensor(f"ps{i}", [128, 512], F32)) for i in range(8)]
        self.psb = [self.B(f"ps{i}") for i in range(8)]
        self.pi = 0
        self.stats_ready = None
        self.bg_gen = None
        self.ones_bf = self.sb("ones_bf", [128, 128], BF16)
        self.ones_f = self.sb("ones_f", [128, 128], F32)
        self.b_const = self.B("const")
        self.dve(lambda e: e.memset(self.ones_bf[:], 1.0), w=[self.b_const])
        self.dve(lambda e: e.memset(self.ones_f[:], 1.0), w=[self.b_const])
        self.eps_t = self.sb("eps_t", [128, 1], F32)
        self.dve(lambda e: e.memset(self.eps_t[:], EPS), w=[self.b_const])
        self.one_t = self.sb("one_t", [128, 1], F32)
        self.dve(lambda e: e.memset(self.one_t[:], 1.0), w=[self.b_const])
        self.zero_t = self.sb("zero_t", [128, 1], F32)
        self.dve(lambda e: e.memset(self.zero_t[:], 0.0), w=[self.b_const])

    def mk_cache(self, name, ntiles, nel):
        return dict(ap=self.dram(name, [ntiles, 128, nel], BF16, "Internal"),
                    bufs=[self.B(f"{name}{j}") for j in range(ntiles)], done=[False] * ntiles)

    def wload(self, src, nel, cache=None, j=None):
        i = self.wi % NWS
        self.wi += 1
        slot, buf = self.wslots[i], self.wbufs[i]
        if cache is not None and cache["done"][j]:
            self.dma(slot[:, 0:nel], cache["ap"][j], r=[cache["bufs"][j]], w=[buf], q="pool", stream=f"w{i}")
            return slot, buf
        self.dma(slot[:, 0:nel], src, w=[buf], q="pool", stream=f"w{i}")
        if cache is not None:
            self.dma(cache["ap"][j], slot[:, 0:nel], r=[buf], w=[cache["bufs"][j]], q="sp", stream=f"wc{i}")
            cache["done"][j] = True
        return slot, buf

    def bank(self):
        i = self.pi % 6
        self.pi += 1
        return self.psum[i], self.psb[i]

    def mod_alloc(self, l):
        return dict(cs=self.sb(f"c_sb{l}", [128, NKC, 2], F32), csil=self.sb(f"c_sil{l}", [128, NKC, 2], BF16),
                    bm=self.sb(f"bm{l}", [128, 144], F32))

    def mod_phase_gen(self, l, wmod, bmodT, cT, modT, bmod_buf, tl):
        cs, csil, bm = tl["cs"], tl["csil"], tl["bm"]
        csb = self.B("c_sb")
        self.dma(cs[:], cT, w=[csb], stream=f"cT{l}")
        csilb = self.B("csil")
        self.act(lambda e: e.activation(out=csil[:], in_=cs[:], func=AF.Silu), r=[csb], w=[csilb])
        bmb = self.B("bm")
        self.dma(bm[:], bmodT, w=[bmb], stream=f"bm{l}")
        ps, psb = self.psum[6], self.psb[6]
        psv = ps[:, 0:288].rearrange("p (j k) -> p j k", k=2)
        for t in range(36):
            slot, sbuf_ = self.wload(wmod[t // 18][t % 18], NKC * 512)
            sv = slot[:, 0:NKC * 512].rearrange("p (k n) -> p k n", n=512)
            for j in range(4):
                col = t * 4 + j
                for kc in range(NKC):
                    self.pe(lambda e, col=col, kc=kc, j=j, sv=sv: e.matmul(
                        psv[:, col, :], lhsT=sv[:, kc, j * 128:(j + 1) * 128], rhs=csil[:, kc, :],
                        start=(kc == 0), stop=(kc == NKC - 1)),
                        r=[sbuf_, csilb], w=[psb])
            if t < 35:
                yield
        for col in range(2):
            self.dve(lambda e, col=col: e.tensor_tensor(out=modT[:, :, col], in0=psv[:, :, col], in1=bm[:],
                                                        op=ALU.add), r=[psb, bmb], w=[bmod_buf])

    def mod_phase(self, l, wmod, bmodT, cT, modT, bmod_buf, tl=None):
        for _ in self.mod_phase_gen(l, wmod, bmodT, cT, modT, bmod_buf, tl or self.mod_alloc(l)):
            pass

    def mod_consts_alloc(self, l, tag):
        return [(self.sb(f"A_{tag}_{l}_{sl}", [128, NKC], F32), self.sb(f"G_{tag}_{l}_{sl}", [128, NKC], F32),
                 self.sb(f"S_{tag}_{l}_{sl}", [128, NKC], F32)) for sl in range(3)]

    def mod_consts(self, l, modT, modb, normgT, ngb, col, tag, tiles=None):
        res = []
        tiles = tiles or self.mod_consts_alloc(l, tag)
        for sl in range(3):
            A, G, Sh = tiles[sl]
            b = self.B("modc")
            sc = modT[:, (3 * sl + 1) * 16:(3 * sl + 2) * 16, col]
            gt = modT[:, (3 * sl + 2) * 16:(3 * sl + 3) * 16, col]
            ng = normgT[:, (l * 3 + sl) * 16:(l * 3 + sl + 1) * 16]
            self.dve(lambda e, A=A, sc=sc, ng=ng: e.scalar_tensor_tensor(
                out=A[:], in0=sc, scalar=1.0, in1=ng, op0=ALU.add, op1=ALU.mult), r=[modb, ngb], w=[b])
            gs = 0.5 if sl != 1 else 1.0
            self.dve(lambda e, G=G, gt=gt, gs=gs: e.tensor_scalar(
                out=G[:], in0=gt, scalar1=gs, scalar2=None, op0=ALU.mult), r=[modb], w=[b])
            sh = modT[:, (3 * sl) * 16:(3 * sl + 1) * 16, col]
            self.dve(lambda e, Sh=Sh, sh=sh: e.tensor_copy(out=Sh[:], in_=sh), r=[modb], w=[b])
            res.append((A, Sh, G, b))
        return res

    def alloc_tile_state(self):
        self.xT = self.sb("xT", [128, NKC, NT], F32)
        self.xb = [self.B(f"xT{c}") for c in range(NKC)]
        self.hT = self.sb("hT", [128, NKC, NT], BF16)
        self.hb = [self.B(f"hT{c}") for c in range(NKC)]
        self.R = self.sb("R", [128, NFC, NT], BF16)
        self.Rb = [self.B(f"R{c}") for c in range(NFC)]
        self.sq = [self.sb(f"sq{i}", [128, NT], BF16) for i in range(2)]
        self.sqb = [self.B(f"sq{i}") for i in range(2)]
        self.tmp = [self.sb(f"tmp{i}", [128, NT], F32) for i in range(3)]
        self.tmpb = [self.B(f"tmp{i}") for i in range(3)]
        self.ti = 0
        self.rstd = self.sb("rstd", [128, NT], F32)
        self.rstdb = self.B("rstd")
        self.sqr = self.sb("sqr", [128, NT], F32)
        self.sqrb = self.B("sqr")

    def gettmp(self):
        i = self.ti % 3
        self.ti += 1
        return self.tmp[i], self.tmpb[i]

    def stat_square(self, c, W):
        sq, sqb = self.sq[c % 2], self.sqb[c % 2]
        xT = self.xT
        self.act(lambda e, c=c, sq=sq, xT=xT: e.activation(out=sq[:, :W], in_=xT[:, c, :W], func=AF.Square),
                 r=[self.xb[c]], w=[sqb])
        return (c, sq, sqb)

    def stat_accum(self, pend, W):
        c, sq, sqb = pend
        ps, psb = self.psum[7], self.psb[7]
        self.pe(lambda e, c=c, sq=sq: e.matmul(ps[:, :W], lhsT=self.ones_bf[:], rhs=sq[:, :W],
                                              start=(c == 0), stop=(c == NKC - 1)),
                r=[sqb, self.b_const], w=[psb])

    def rms_stats(self, W):
        xT = self.xT
        ps, psb = self.psum[7], self.psb[7]
        if self.stats_ready == W:
            self.stats_ready = None
        else:
            for c in range(NKC):
                self.stat_accum(self.stat_square(c, W), W)
        sqr, rstd = self.sqr, self.rstd
        self.act(lambda e: e.activation(out=sqr[:, :W], in_=ps[:, :W], func=AF.Sqrt,
                                        bias=self.eps_t[:], scale=1.0 / D),
                 r=[psb, self.b_const], w=[self.sqrb])
        self.dve(lambda e: e.reciprocal(out=rstd[:, :W], in_=sqr[:, :W]), r=[self.sqrb], w=[self.rstdb])

    def modulate(self, W, mc):
        A, Sh, G, mb = mc
        xT, hT = self.xT, self.hT
        rstd = self.rstd
        for c in range(NKC):
            t, tb = self.gettmp()
            self.dve(lambda e, c=c, t=t: e.scalar_tensor_tensor(
                out=t[:, :W], in0=xT[:, c, :W], scalar=A[:, c:c + 1], in1=rstd[:, :W],
                op0=ALU.mult, op1=ALU.mult), r=[self.xb[c], mb, self.rstdb], w=[tb])
            self.act(lambda e, c=c, t=t: e.activation(out=hT[:, c, :W], in_=t[:, :W], func=AF.Identity,
                                                      bias=Sh[:, c:c + 1], scale=1.0),
                     r=[tb, mb], w=[self.hb[c]])

    def ffn(self, W, mc, w_in, w_out, post_stats=True, c_in=None, c_out=None):
        A, Sh, G, mb = mc
        xT, hT, R = self.xT, self.hT, self.R
        self.rms_stats(W)
        self.modulate(W, mc)
        for i in range(NFC):
            slot, sbuf_ = self.wload(w_in[i], NKC * 256, c_in, i)
            sv = slot[:, 0:NKC * 256].rearrange("p (k n) -> p k n", n=256)
            pg, pgb = self.bank()
            pu, pub = self.bank()
            for half, (ps, psb) in enumerate(((pg, pgb), (pu, pub))):
                for kc in range(NKC):
                    self.pe(lambda e, ps=ps, sv=sv, half=half, kc=kc: e.matmul(
                        ps[:, :W], lhsT=sv[:, kc, half * 128:(half + 1) * 128], rhs=hT[:, kc, :W],
                        start=(kc == 0), stop=(kc == NKC - 1)), r=[sbuf_, self.hb[kc]], w=[psb])
            t, tb = self.gettmp()
            self.act(lambda e, t=t, pg=pg: e.activation(out=t[:, :W], in_=pg[:, :W], func=AF.Silu),
                     r=[pgb], w=[tb])
            self.dve(lambda e, t=t, pu=pu, i=i: e.tensor_tensor(out=R[:, i, :W], in0=t[:, :W], in1=pu[:, :W],
                                                              op=ALU.mult), r=[tb, pub], w=[self.Rb[i]])
        pend = None
        for m in range(NKC):
            slot, sbuf_ = self.wload(w_out[m], NFC * 128, c_out, m)
            sv = slot[:, 0:NFC * 128].rearrange("p (k n) -> p k n", n=128)
            ps, psb = self.bank()
            for k in range(NFC):
                self.pe(lambda e, ps=ps, sv=sv, k=k: e.matmul(
                    ps[:, :W], lhsT=sv[:, k, :], rhs=R[:, k, :W], start=(k == 0), stop=(k == NFC - 1)),
                    r=[sbuf_, self.Rb[k]], w=[psb])
            if pend is not None:
                self.stat_accum(pend, W)
                pend = None
            self.dve(lambda e, ps=ps, m=m: e.scalar_tensor_tensor(
                out=xT[:, m, :W], in0=ps[:, :W], scalar=G[:, m:m + 1], in1=xT[:, m, :W],
                op0=ALU.mult, op1=ALU.add), r=[psb, mb, self.xb[m]], w=[self.xb[m]])
            if post_stats:
                pend = self.stat_square(m, W)
        if pend is not None:
            self.stat_accum(pend, W)
        if post_stats:
            self.stats_ready = W

    def load_xT(self, src, W):
        self.dma(self.xT[:, :, :W], src.rearrange("c p t -> p c t"), w=self.xb, stream="xTld")

    def store_xT(self, dst, W, wb=()):
        self.dma(dst.rearrange("c p t -> p c t"), self.xT[:, :, :W], r=self.xb, w=list(wb), stream="xTst")

    def finish(self, out_ops_bufs):
        self.P.add("sp", None, reads=out_ops_bufs, writes=())
        self.P.emit()
        self.es.close()
        return self.nc


def _tiles_cols(w, col_lists):
    K = w.shape[0]
    kc = K // 128
    w3 = w.reshape(kc, 128, w.shape[1])
    out = []
    for cols in col_lists:
        t = w3[:, :, cols]
        out.append(np.ascontiguousarray(t.transpose(1, 0, 2)).reshape(128, -1))
    return np.stack(out, 0)


def _vecT(v):
    return np.ascontiguousarray(v.reshape(-1, 128).T)


def prep_shared(inp):
    sh = {}
    sh["wmod"] = [_tiles_cols(inp["w_mod"][l], [np.arange(t * 512, (t + 1) * 512) for t in range(36)])
                  for l in range(2)]
    sh["bmodT"] = [_vecT(inp["b_mod"][l]) for l in range(2)]
    sh["normgT"] = np.concatenate([_vecT(inp["norm_g"][l, s]) for l in range(2) for s in range(3)], axis=1)
    sh["fnormgT"] = _vecT(inp["final_norm_g"])
    ffn_in, ffn_out = [], []
    for l in range(2):
        for j in range(2):
            wi = inp["ffn_w_in"][l, j]
            ffn_in.append(_tiles_cols(wi, [np.concatenate([np.arange(i * 128, (i + 1) * 128),
                                                           DFF + np.arange(i * 128, (i + 1) * 128)])
                                           for i in range(NFC)]))
            wo = inp["ffn_w_out"][l, j]
            ffn_out.append(_tiles_cols(wo, [np.arange(m * 128, (m + 1) * 128) for m in range(NKC)]))
    sh["ffn_in"] = ffn_in
    sh["ffn_out"] = ffn_out
    return sh


def _barrier(self):
    lasts = []
    for lst in self.P.stream_ops.values():
        for i in reversed(lst):
            if self.P.ops[i].fn is not None:
                lasts.append(i)
                break
    for e in Prog.ENGS:
        op = self.P.add(e, None)
        op.deps = set(lasts)


def _Rbufs(self, off, n):
    return self.Rb[off // NT:(off + n + NT - 1) // NT]


def _mlstm_proj(self, W, mc, c0, dd, kind):
    nsub = W // 128
    hT = self.hT
    is_ctx = kind != "own"
    self.rms_stats(W)
    self.modulate(W, mc)
    Rf = self.R[:].rearrange("p c t -> p (c t)")
    qst = Rf[:, 0:4096].rearrange("p (s h j) -> p s h j", s=4, h=8)
    kst = Rf[:, 4096:8192].rearrange("p (s h j) -> p s h j", s=4, h=8)
    ktm = Rf[:, 8192:12288].rearrange("p (s n) -> p s n", s=4)
    osg = Rf[:, 12288:20480].rearrange("p (s n) -> p s n", s=4)
    qb, kb, ktb, ob = self.Rbufs(0, 4096), self.Rbufs(4096, 4096), self.Rbufs(8192, 4096), self.Rbufs(12288, 8192)
    Vst, Vb = self.Vst, self.Vstb
    if not is_ctx:
        for i in range(8):
            slot, sbuf_ = self.wload(dd["wqk"][i], NKC * 256, dd.get("c_wqk"), i)
            sv = slot[:, 0:NKC * 256].rearrange("p (k n) -> p k n", n=256)
            for half in range(2):
                ps, psb = self.bank()
                for kc in range(NKC):
                    self.pe(lambda e, ps=ps, sv=sv, half=half, kc=kc: e.matmul(
                        ps[:, :W], lhsT=sv[:, kc, half * 128:(half + 1) * 128], rhs=hT[:, kc, :W],
                        start=(kc == 0), stop=(kc == NKC - 1)), r=[sbuf_, self.hb[kc]], w=[psb])
                hh = (i % 4) * 2 + half
                dst, dstb, scale = (qst, qb, DK ** -0.5) if i < 4 else (kst, kb, 1.0)
                self.act(lambda e, ps=ps, dst=dst, hh=hh, scale=scale: e.activation(
                    out=dst[:, 0:nsub, hh, :], in_=ps[:, :W].rearrange("p (s j) -> p s j", j=128),
                    func=AF.Copy, scale=scale), r=[psb], w=dstb)
    for i in range(10):
        if is_ctx and i >= 6:
            continue
        slot, sbuf_ = self.wload(dd["wtm"][i], NKC * 512, dd.get("c_wtm"), i)
        sv = slot[:, 0:NKC * 512].rearrange("p (k n) -> p k n", n=512)
        for sub in range(nsub):
            ps, psb = self.bank()
            for kc in range(NKC):
                self.pe(lambda e, ps=ps, sv=sv, sub=sub, kc=kc: e.matmul(
                    ps[:, :], lhsT=hT[:, kc, sub * 128:(sub + 1) * 128], rhs=sv[:, kc, :],
                    start=(kc == 0), stop=(kc == NKC - 1)), r=[sbuf_, self.hb[kc]], w=[psb])
            if i < 2:
                self.dve(lambda e, ps=ps, sub=sub, i=i: e.tensor_copy(out=ktm[:, sub, i * 512:(i + 1) * 512],
                                                                   in_=ps[:, :]), r=[psb], w=ktb)
            elif i < 6:
                vi = i - 2
                self.dve(lambda e, ps=ps, sub=sub, vi=vi: e.tensor_copy(
                    out=Vst[:, sub, 2 * vi:2 * vi + 2, 0:DV], in_=ps[:, :].rearrange("p (h e) -> p h e", e=DV)),
                    r=[psb], w=[Vb])
            else:
                oi = i - 6
                self.act(lambda e, ps=ps, sub=sub, oi=oi: e.activation(
                    out=osg[:, sub, oi * 512:(oi + 1) * 512], in_=ps[:, :], func=AF.Sigmoid), r=[psb], w=ob)
    slot, sbuf_ = self.wload(dd["wgate"], NKC * 128, dd.get("c_wgate"), 0)
    sv = slot[:, 0:NKC * 128].rearrange("p (k n) -> p k n", n=128)
    pi_, pib = self.bank()
    pf_, pfb = self.bank()
    for half, (ps, psb) in enumerate(((pi_, pib), (pf_, pfb))):
        for kc in range(NKC):
            self.pe(lambda e, ps=ps, sv=sv, half=half, kc=kc: e.matmul(
                ps[0:64, :W], lhsT=sv[:, kc, half * 64:(half + 1) * 64], rhs=hT[:, kc, :W],
                start=(kc == 0), stop=(kc == NKC - 1)), r=[sbuf_, self.hb[kc]], w=[psb])
    Li, Lf, gb = (self.Li_c, self.Lf_c, self.gcb) if kind == "ctx" else (self.Li_x, self.Lf_x, self.gxb)
    t0 = 0 if kind == "ctx" else c0 * 128
    self.act(lambda e: e.activation(out=Li[0:64, t0:t0 + W], in_=pi_[0:64, :W], func=AF.Identity,
                                    bias=self.bg[0:64, 0:1], scale=1.0), r=[pib, self.bgb], w=[gb])
    t, tb = self.gettmp()
    self.act(lambda e, t=t: e.activation(out=t[0:64, :W], in_=pf_[0:64, :W], func=AF.Exp,
                                         bias=self.bg[0:64, 2:3], scale=-1.0), r=[pfb, self.bgb], w=[tb])
    self.act(lambda e, t=t: e.activation(out=t[0:64, :W], in_=t[0:64, :W], func=AF.Ln,
                                         bias=self.one_t[0:64, :], scale=1.0), r=[tb, self.b_const], w=[tb])
    self.dve(lambda e, t=t: e.tensor_scalar(out=Lf[0:64, t0:t0 + W], in0=t[0:64, :W], scalar1=-1.0, scalar2=None,
                                            op0=ALU.mult), r=[tb], w=[gb])
    sfx = {"ctx": "c", "other": "o", "own": "x"}[kind]
    if not is_ctx:
        self.dma(dd["qT_s"][c0:c0 + nsub].rearrange("c p n -> p c n"), Rf[:, 0:nsub * 1024].rearrange("p (s n) -> p s n", s=nsub),
                 r=qb, w=[dd["b_qT"]], stream="st_q")
        self.dma(dd["kT_s"][c0:c0 + nsub].rearrange("c p n -> p c n"), Rf[:, 4096:4096 + nsub * 1024].rearrange("p (s n) -> p s n", s=nsub),
                 r=kb, w=[dd["b_kT"]], stream="st_k")
        self.dma(dd["osig_s"][c0:c0 + nsub].rearrange("c p n -> p c n"), osg[:, 0:nsub, :], r=ob, w=[dd["b_osig"]], stream="st_o")
    self.dma(dd["ktm_" + sfx][c0:c0 + nsub].rearrange("c p n -> p c n"), ktm[:, 0:nsub, :], r=ktb, w=[dd["b_ktm" + sfx]], stream="st_kt")
    self.dma(dd["V_" + sfx][c0:c0 + nsub].rearrange("c p n -> p c n"),
             Vst[:, 0:nsub].rearrange("p s h e -> p s (h e)"), r=[Vb], w=[dd["b_V" + sfx]], stream="st_v")


def _gate_prep(self, tag, Li, Lf, gb, NC, d, start_kind, out):
    rs = slice(0, 8) if d == 0 else slice(32, 40)
    A64 = slice(0, 64)
    g = self.gp
    bF, u, aE, cE = g["bF"], g["u"], g["aE"], g["cE"]
    tot, Am, M, mB, wI = g["tot"], g["A"], g["M"], g["mB"], g["wI"]
    b = g["b"]
    order = list(range(NC)) if d == 0 else list(range(NC - 1, -1, -1))
    v3 = lambda t: t[A64, 0:NC * 128].rearrange("p (c j) -> p c j", j=128)
    bF3, u3, aE3, cE3 = v3(bF), v3(u), v3(aE), v3(cE)
    Li3 = Li[A64, 0:NC * 128].rearrange("p (c j) -> p c j", j=128)
    Lf3 = Lf[A64, 0:NC * 128].rearrange("p (c j) -> p c j", j=128)
    for c in range(NC):
        self.dve(lambda e, c=c: e.tensor_tensor_scan(out=bF3[:, c, :], data0=self.ones_f[A64, :], data1=Lf3[:, c, :],
                                                     initial=0.0, op0=ALU.mult, op1=ALU.add),
                 r=[gb, self.b_const], w=[b])
    self.dve(lambda e: e.tensor_copy(out=tot[A64, 0:NC], in_=bF3[:, :, 127]), r=[b], w=[b])
    if d == 1:
        r1 = slice(32, 64)
        self.dve(lambda e: e.tensor_tensor(out=bF3[r1], in0=Lf3[r1], in1=bF3[r1], op=ALU.subtract), r=[b, gb], w=[b])
        self.dve(lambda e: e.tensor_tensor(out=bF3[r1], in0=bF3[r1],
                                           in1=tot[r1, 0:NC].unsqueeze(2).broadcast_to([32, NC, 128]), op=ALU.add),
                 r=[b], w=[b])
    self.dve(lambda e: e.tensor_tensor(out=u3, in0=Li3, in1=bF3, op=ALU.subtract), r=[b, gb], w=[b])
    self.dve(lambda e: e.tensor_reduce(out=Am[A64, 0:NC], in_=u3, axis=AX.X, op=ALU.max), r=[b], w=[b])
    for k, c in enumerate(order):
        if k == 0 and start_kind == "empty":
            self.dve(lambda e, c=c: e.tensor_copy(out=M[rs, c:c + 1], in_=Am[rs, c:c + 1]), r=[b], w=[b])
            self.dve(lambda e, c=c: e.tensor_copy(out=mB[rs, c:c + 1], in_=Am[rs, c:c + 1]), r=[b], w=[b])
        else:
            self.dve(lambda e, c=c: e.tensor_tensor(out=M[rs, c:c + 1], in0=mB[rs, c:c + 1], in1=Am[rs, c:c + 1],
                                                    op=ALU.max), r=[b], w=[b])
        dst = mB[rs, order[k + 1]:order[k + 1] + 1] if k + 1 < NC else g["mfin"][rs, 0:1]
        self.dve(lambda e, c=c, dst=dst: e.tensor_tensor(out=dst, in0=M[rs, c:c + 1], in1=tot[rs, c:c + 1], op=ALU.add),
                 r=[b], w=[b])
    self.dve(lambda e: e.tensor_tensor(out=wI[A64, 0:NC], in0=mB[A64, 0:NC], in1=M[A64, 0:NC], op=ALU.subtract), r=[b], w=[b])
    self.act(lambda e: e.activation(out=wI[A64, 0:NC], in_=wI[A64, 0:NC], func=AF.Exp), r=[b], w=[b])
    Mb = M[A64, 0:NC].unsqueeze(2).broadcast_to([64, NC, 128])
    self.dve(lambda e: e.tensor_tensor(out=aE3, in0=u3, in1=Mb, op=ALU.subtract), r=[b], w=[b])
    self.act(lambda e: e.activation(out=aE3, in_=aE3, func=AF.Exp), r=[b], w=[b])
    self.dve(lambda e: e.scalar_tensor_tensor(out=cE3, in0=bF3, scalar=-1.0, in1=Mb, op0=ALU.mult, op1=ALU.subtract),
             r=[b], w=[b])
    self.act(lambda e: e.activation(out=cE3, in_=cE3, func=AF.Exp), r=[b], w=[b])
    idn = self.cst[0:64, 256:320]
    for src3, dst in ((aE3, out["aT"]), (cE3, out["clT"])):
        for half in range((NC + 7) // 8):
            ps, psb = self.bank()
            n = min(8, NC - half * 8)
            for cc in range(n):
                c = half * 8 + cc
                self.pe(lambda e, ps=ps, cc=cc, c=c, src3=src3: e.matmul(
                    ps[:, cc * 64:(cc + 1) * 64], lhsT=src3[rs, c, :], rhs=idn[rs, :], start=True, stop=True),
                    r=[b, self.cstb], w=[psb])
            self.dve(lambda e, ps=ps, n=n, half=half, dst=dst: e.tensor_copy(
                out=dst[:, half * 8:half * 8 + n, :],
                in_=ps[:, 0:n * 64].rearrange("p (c r) -> p c r", r=64)[:, :, rs]), r=[psb], w=[out["b"]])
    for half in range((NC + 7) // 8):
        n = min(8, NC - half * 8)
        t, tb = self.gettmp()
        t3 = t[0:64, 0:n * 64].rearrange("p (c r) -> p c r", r=64)
        self.dve(lambda e, t3=t3, n=n, half=half: e.tensor_tensor(
            out=t3, in0=wI[A64, half * 8:half * 8 + n].unsqueeze(2).broadcast_to([64, n, 64]),
            in1=idn.unsqueeze(1).broadcast_to([64, n, 64]), op=ALU.mult), r=[b, self.cstb], w=[tb])
        ps, psb = self.bank()
        self.pe(lambda e, ps=ps, t=t, n=n: e.matmul(ps[:, 0:n * 64], lhsT=self.ones_f[rs, :], rhs=t[rs, 0:n * 64],
                                                    start=True, stop=True), r=[tb, self.b_const], w=[psb])
        self.dve(lambda e, ps=ps, n=n, half=half: e.tensor_copy(
            out=out["wB"][:, half * 8:half * 8 + n, :],
            in_=ps[:, 0:n * 64].rearrange("p (c r) -> p c r", r=64)[:, :, rs]), r=[psb], w=[out["b"]])


def _scan(self, NC, d, gp, dd, sfx, state_only, start_empty, accumulate):
    order = list(range(NC)) if d == 0 else list(range(NC - 1, -1, -1))
    s = self.sc
    C, Cbuf = self.C, self.Cb_
    C3 = C[:].rearrange("p (h e) -> p h e", e=DVE_)
    mask = self.cst[:, d * 128:(d + 1) * 128]
    def loads(k):
        c = order[k]
        i2 = k % 2
        lb = s["lb"][i2]
        if not state_only:
            self.dma(s["q"][i2][:], dd["qT_s"][c], r=[dd["b_qT"]], w=[lb[0]], stream=f"ld_q{i2}")
            self.dma(s["k"][i2][:], dd["kT_s"][c], r=[dd["b_kT"]], w=[lb[1]], stream=f"ld_k{i2}")
        self.dma(s["kt"][i2][:], dd["ktm_" + sfx][c], r=[dd["b_ktm" + sfx]], w=[lb[2]], stream=f"ld_kt{i2}")
        self.dma(s["V"][i2][:], dd["V_" + sfx][c], r=[dd["b_V" + sfx]], w=[lb[3]], stream=f"ld_v{i2}")
        if accumulate:
            self.dma(s["H1"][i2][:], dd["H_rd"][c], r=[dd["b_Hin"]], w=[s["H1b"][i2]], stream=f"ld_h{i2}")

    loads(0)
    for k, c in enumerate(order):
        i2 = k % 2
        if k + 1 < NC:
            loads(k + 1)
        qc, kc_, ktc, Vc = s["q"][i2], s["k"][i2], s["kt"][i2], s["V"][i2]
        lb = s["lb"][i2]
        if accumulate:
            H1 = s["H1"][i2]
        Vt, Vtb = s["Vt"], s["Vtb"]
        V3 = Vc[:].rearrange("p (h e) -> p h e", e=DVE_)
        Vt3 = Vt[:].rearrange("p (h e) -> p h e", e=DVE_)
        self.dve(lambda e, V3=V3, c=c: e.tensor_tensor(
            out=Vt3, in0=V3, in1=gp["aT"][:, c, :].unsqueeze(2).broadcast_to([128, 8, DVE_]), op=ALU.mult),
            r=[lb[3], gp["b"]], w=[Vtb])
        first = (k == 0 and start_empty)
        if not state_only and not first:
            Cbf, Cbfb = s["Cbf"], s["Cbfb"]
            Cbf3 = Cbf[:].rearrange("p (h e) -> p h e", e=DVE_)
            self.dve(lambda e, c=c: e.tensor_tensor(
                out=Cbf3, in0=C3, in1=gp["wB"][:, c, :].unsqueeze(2).broadcast_to([128, 8, DVE_]), op=ALU.mult),
                r=[Cbuf, gp["b"]], w=[Cbfb])
        if not state_only:
            Ho, Hob = s["Ho"][i2], s["Hob"][i2]
        LA = 2
        pSd = {}

        def emit_S(h):
            pS, pSb = self.bank()
            self.pe(lambda e, pS=pS, h=h, qc=qc, kc_=kc_: e.matmul(
                pS[:, 0:128], lhsT=kc_[:, h * 128:(h + 1) * 128], rhs=qc[:, h * 128:(h + 1) * 128],
                start=True, stop=True), r=[lb[0], lb[1]], w=[pSb])
            pSd[h] = (pS, pSb)

        def emit_mask(h):
            pS, pSb = pSd[h]
            St, Stb = s["St"][h % 4], s["Stb"][h % 4]
            self.dve(lambda e, pS=pS, St=St: e.tensor_tensor(out=St[:], in0=pS[:, 0:128], in1=mask, op=ALU.mult),
                     r=[pSb, self.cstb], w=[Stb])

        if not state_only:
            for h in range(min(LA, H)):
                emit_S(h)
                emit_mask(h)
        for h in range(H):
            if not state_only:
                if h + LA < H:
                    emit_S(h + LA)
                St, Stb = s["St"][h % 4], s["Stb"][h % 4]
                pH, pHb = self.bank()
                self.pe(lambda e, pH=pH, St=St, h=h, first=first: e.matmul(
                    pH[:, 0:DVE_], lhsT=St[:], rhs=Vt3[:, h, :], start=True, stop=first), r=[Stb, Vtb], w=[pHb])
                if not first:
                    self.pe(lambda e, pH=pH, h=h, qc=qc: e.matmul(
                        pH[:, 0:DVE_], lhsT=qc[:, h * 128:(h + 1) * 128], rhs=Cbf3[:, h, :], start=False, stop=True),
                        r=[lb[0], Cbfb], w=[pHb])
            pK, pKb = self.bank()
            self.pe(lambda e, pK=pK, h=h, ktc=ktc: e.matmul(
                pK[:, 0:DVE_], lhsT=ktc[:, h * 128:(h + 1) * 128], rhs=Vt3[:, h, :], start=True, stop=True),
                r=[lb[2], Vtb], w=[pKb])
            if not state_only:
                if h + LA < H:
                    emit_mask(h + LA)
                dn, dnb = s["dn"][h % 2], s["dnb"][h % 2]
                self.dve(lambda e, pH=pH, dn=dn: e.tensor_scalar(
                    out=dn[:, 1:2], in0=pH[:, DV:DVE_], scalar1=-1.0, scalar2=None, op0=ALU.mult),
                    r=[pHb], w=[dnb])
                self.dve(lambda e, pH=pH, dn=dn, c=c, h=h: e.scalar_tensor_tensor(
                    out=dn[:, 0:1], in0=pH[:, DV:DVE_], scalar=gp["clT"][:, c, h:h + 1], in1=dn[:, 1:2],
                    op0=ALU.max, op1=ALU.max), r=[pHb, gp["b"], dnb], w=[dnb])
                self.dve(lambda e, dn=dn: e.reciprocal(out=dn[:, 1:2], in_=dn[:, 0:1]), r=[dnb], w=[dnb])
                if accumulate:
                    self.dve(lambda e, pH=pH, dn=dn, h=h, Ho=Ho, H1=H1: e.scalar_tensor_tensor(
                        out=Ho[:, h * DV:(h + 1) * DV], in0=pH[:, 0:DV], scalar=dn[:, 1:2],
                        in1=H1[:, h * DV:(h + 1) * DV], op0=ALU.mult, op1=ALU.add),
                        r=[pHb, dnb, s["H1b"][i2]], w=[Hob])
                else:
                    self.act(lambda e, pH=pH, dn=dn, h=h, Ho=Ho: e.activation(
                        out=Ho[:, h * DV:(h + 1) * DV], in_=pH[:, 0:DV], func=AF.Copy, scale=dn[:, 1:2]),
                        r=[pHb, dnb], w=[Hob])
            if first:
                self.dve(lambda e, pK=pK, h=h: e.tensor_copy(out=C3[:, h, :], in_=pK[:, 0:DVE_]), r=[pKb], w=[Cbuf])
            else:
                self.dve(lambda e, pK=pK, h=h, c=c: e.scalar_tensor_tensor(
                    out=C3[:, h, :], in0=C3[:, h, :], scalar=gp["wB"][:, c, h:h + 1], in1=pK[:, 0:DVE_],
                    op0=ALU.mult, op1=ALU.add), r=[pKb, gp["b"], Cbuf], w=[Cbuf])
        if not state_only:
            self.dma(dd["H_wr"][c], Ho[:], r=[Hob], w=[dd["b_Hout"]], stream=f"st_h{i2}")
        if self.bg_gen is not None:
            if next(self.bg_gen, "done") == "done":
                self.bg_gen = None


Builder.barrier = _barrier
Builder.Rbufs = _Rbufs
Builder.mlstm_proj = _mlstm_proj
Builder.gate_prep = _gate_prep
Builder.scan = _scan


def _mixer_out(self, W, c0, G, mb, dd):
    nsub = W // 128
    xT, hT = self.xT, self.hT
    idb = self.identb
    for sub in range(nsub):
        c = c0 + sub
        i2 = 0
        Hc, Hcb = self.Hc[i2], self.Hcb[i2]
        og, ogb = self.og[i2], self.ogb[i2]
        self.dma(Hc[:], dd["H_fin"][c], r=[dd["b_Hout"]], w=[Hcb], stream=f"ld_H{i2}")
        self.dma(og[:], dd["osig_s"][c], r=[dd["b_osig"]], w=[ogb], stream=f"ld_og{i2}")
        sqf, sqfb = self.sqf, self.sqfb
        self.dve(lambda e, Hc=Hc: e.tensor_tensor(out=sqf[:], in0=Hc[:], in1=Hc[:], op=ALU.mult), r=[Hcb], w=[sqfb])
        ss, ssb = self.ss, self.ssb
        self.dve(lambda e: e.tensor_reduce(out=ss[:, 0:8], in_=sqf[:].rearrange("p (h e) -> p h e", e=DV),
                                           axis=AX.X, op=ALU.add), r=[sqfb], w=[ssb])
        self.act(lambda e: e.activation(out=ss[:, 8:16], in_=ss[:, 0:8], func=AF.Sqrt, bias=self.eps_t[:],
                                        scale=1.0 / DV), r=[ssb, self.b_const], w=[ssb])
        self.dve(lambda e: e.reciprocal(out=ss[:, 16:24], in_=ss[:, 8:16]), r=[ssb], w=[ssb])
        self.dve(lambda e, Hc=Hc: e.tensor_tensor(
            out=sqf[:].rearrange("p (h e) -> p h e", e=DV), in0=Hc[:].rearrange("p (h e) -> p h e", e=DV),
            in1=ss[:, 16:24].unsqueeze(2).broadcast_to([128, 8, DV]), op=ALU.mult), r=[Hcb, ssb, sqfb], w=[sqfb])
        self.dve(lambda e: e.tensor_tensor(out=sqf[:], in0=sqf[:], in1=self.gN[:], op=ALU.mult),
                 r=[sqfb, self.gNb], w=[sqfb])
        yb = self.R[:].rearrange("p c t -> p (c t)")[:, 0:D]
        ybl = self.Rb[0:4]
        self.dve(lambda e, og=og, yb=yb: e.tensor_tensor(out=yb, in0=sqf[:], in1=og[:], op=ALU.mult),
                 r=[sqfb, ogb], w=ybl)
        for g4 in range(4):
            ps, psb = self.bank()
            psv = ps[:].bitcast(BF16)
            for j in range(4):
                kc = g4 * 4 + j
                self.pe(lambda e, psv=psv, j=j, kc=kc, yb=yb: e.transpose(
                    out=psv[:, j * 128:(j + 1) * 128], in_=yb[:, kc * 128:(kc + 1) * 128], identity=idb[:]),
                    r=ybl + [self.cstb], w=[psb])
            self.act(lambda e, psv=psv, g4=g4, sub=sub: e.activation(
                out=hT[:, g4 * 4:g4 * 4 + 4, sub * 128:(sub + 1) * 128],
                in_=psv[:, 0:512].rearrange("p (k t) -> p k t", t=128), func=AF.Copy),
                r=[psb], w=self.hb[g4 * 4:g4 * 4 + 4])
    self.proj_out(W, dd["mwout"], G, mb, self.hT, self.hb)


def _proj_out(self, W, wtiles, G, mb, src, srcb):
    xT = self.xT
    pend = None
    for i in range(8):
        slot, sbuf_ = self.wload(wtiles[i], NKC * 256)
        sv = slot[:, 0:NKC * 256].rearrange("p (k n) -> p k n", n=256)
        for half in range(2):
            m = 2 * i + half
            ps, psb = self.bank()
            for kc in range(NKC):
                self.pe(lambda e, ps=ps, sv=sv, half=half, kc=kc: e.matmul(
                    ps[:, :W], lhsT=sv[:, kc, half * 128:(half + 1) * 128], rhs=src[:, kc, :W],
                    start=(kc == 0), stop=(kc == NKC - 1)), r=[sbuf_, srcb[kc]], w=[psb])
            if pend is not None:
                self.stat_accum(pend, W)
                pend = None
            self.dve(lambda e, ps=ps, m=m: e.scalar_tensor_tensor(
                out=xT[:, m, :W], in0=ps[:, :W], scalar=G[:, m:m + 1], in1=xT[:, m, :W],
                op0=ALU.mult, op1=ALU.add), r=[psb, mb, self.xb[m]], w=[self.xb[m]])
            pend = self.stat_square(m, W)
    self.stat_accum(pend, W)
    self.stats_ready = W


def _conv_mixer(self, W, mc, dd):
    A, Sh, G, mb = mc
    hT, R = self.hT, self.R
    cw = self.cw
    self.rms_stats(W)
    self.modulate(W, mc)
    nr = W // 64
    for i in range(NKC):
        slot, sbuf_ = self.wload(dd["cwin"][i], NKC * 384)
        sv = slot[:, 0:NKC * 384].rearrange("p (k n) -> p k n", n=384)
        pss = []
        for part in range(3):
            ps, psb = self.bank()
            pss.append((ps, psb))
            for kc in range(NKC):
                self.pe(lambda e, ps=ps, sv=sv, part=part, kc=kc: e.matmul(
                    ps[:, :W], lhsT=sv[:, kc, part * 128:(part + 1) * 128], rhs=hT[:, kc, :W],
                    start=(kc == 0), stop=(kc == NKC - 1)), r=[sbuf_, self.hb[kc]], w=[psb])
        (pbg, pbgb), (pcg, pcgb), (pu, pub) = pss
        t1, t1b = self.gettmp()
        self.act(lambda e, t1=t1, pcg=pcg: e.activation(out=t1[:, :W], in_=pcg[:, :W], func=AF.Copy), r=[pcgb], w=[t1b])
        z, zb = self.gettmp()
        self.dve(lambda e, z=z, t1=t1, pu=pu: e.tensor_tensor(out=z[:, :W], in0=t1[:, :W], in1=pu[:, :W], op=ALU.mult),
                 r=[t1b, pub], w=[zb])
        zc, zcb = self.gettmp()
        self.dve(lambda e, z=z, zc=zc, i=i: e.tensor_scalar(out=zc[:, :W], in0=z[:, :W], scalar1=cw[:, i, 1:2],
                                                          scalar2=None, op0=ALU.mult), r=[zb, self.cwb], w=[zcb])
        z3 = z[:, :W].rearrange("p (r j) -> p r j", j=64)
        zc3 = zc[:, :W].rearrange("p (r j) -> p r j", j=64)
        self.dve(lambda e, z3=z3, zc3=zc3, i=i: e.scalar_tensor_tensor(
            out=zc3[:, :, 1:64], in0=z3[:, :, 0:63], scalar=cw[:, i, 0:1], in1=zc3[:, :, 1:64],
            op0=ALU.mult, op1=ALU.add), r=[zb, zcb, self.cwb], w=[zcb])
        self.dve(lambda e, z3=z3, zc3=zc3, i=i: e.scalar_tensor_tensor(
            out=zc3[:, :, 0:63], in0=z3[:, :, 1:64], scalar=cw[:, i, 2:3], in1=zc3[:, :, 0:63],
            op0=ALU.mult, op1=ALU.add), r=[zb, zcb, self.cwb], w=[zcb])
        self.dve(lambda e, zc=zc, pbg=pbg, i=i: e.tensor_tensor(out=R[:, i, :W], in0=pbg[:, :W], in1=zc[:, :W],
                                                              op=ALU.mult), r=[pbgb, zcb], w=[self.Rb[i]])
    self.proj_out(W, dd["cwout"], G, mb, R, self.Rb)


def _final_norm(self, W, fg, fgb):
    xT = self.xT
    rstd = self.rstd
    self.rms_stats(W)
    for c in range(NKC):
        self.dve(lambda e, c=c: e.scalar_tensor_tensor(
            out=xT[:, c, :W], in0=xT[:, c, :W], scalar=fg[:, c:c + 1], in1=rstd[:, :W],
            op0=ALU.mult, op1=ALU.mult), r=[self.xb[c], fgb, self.rstdb], w=[self.xb[c]])


Builder.mixer_out = _mixer_out
Builder.proj_out = _proj_out
Builder.conv_mixer = _conv_mixer
Builder.final_norm = _final_norm


def build_program(mode):
    b = Builder(mode)
    nc = b.nc
    dd = {}
    A_ = mode in "AF"
    B_ = mode in "BF"
    cT_d = b.din("cT", [128, NKC, 2])
    normg_d = b.din("normgT", [128, 96])
    cst_d = b.din("consts", [128, 384])
    if A_:
        xT_d = b.din("xT", [NKC, 128, TOK])
        ctxT_d = b.din("ctxT", [NKC, 128, CTX])
        wmod0 = [b.din("wmod0a", [18, 128, NKC * 512]), b.din("wmod0b", [18, 128, NKC * 512])]
        bmod0 = b.din("bmodT0", [128, 144])
        fin = {0: b.din("ffn_in0", [NFC, 128, NKC * 256])}
        fout = {0: b.din("ffn_out0", [NKC, 128, NFC * 128])}
        dd["wqk"] = b.din("wqk", [8, 128, NKC * 256])
        dd["wtm"] = b.din("wtm", [10, 128, NKC * 512])
        dd["wgate"] = b.din("wgate", [128, NKC * 128])
        bg_d = b.din("bgate", [64, 2])
    if B_:
        wmod1 = [b.din("wmod1a", [18, 128, NKC * 512]), b.din("wmod1b", [18, 128, NKC * 512])]
        bmod1 = b.din("bmodT1", [128, 144])
        if not A_:
            fin, fout = {}, {}
        for i in (1, 2, 3):
            fin[i] = b.din(f"ffn_in{i}", [NFC, 128, NKC * 256])
            fout[i] = b.din(f"ffn_out{i}", [NKC, 128, NFC * 128])
        dd["mwout"] = b.din("mwout", [8, 128, NKC * 256])
        mng_d = b.din("mnormg", [128, D])
        dd["cwin"] = b.din("cwin", [NKC, 128, NKC * 384])
        cwT_d = b.din("cwT", [128, 48])
        dd["cwout"] = b.din("cwout", [8, 128, NKC * 256])
        fng_d = b.din("fnormgT", [128, NKC])
        out_d = b.dout("outT", [NKC, 128, TOK])
    xs = b.scratch("xs", [NKC, 128, TOK], F32, "A")
    dd["qT_s"] = b.scratch("qT_s", [NCH, 128, 1024], BF16, "A")
    dd["kT_s"] = b.scratch("kT_s", [NCH, 128, 1024], BF16, "A")
    dd["ktm_x"] = b.scratch("ktm_x", [NCH, 128, 1024], BF16, "A")
    dd["V_x"] = b.scratch("V_x", [NCH, 128, H * DVE_], BF16, "A")
    dd["osig_s"] = b.scratch("osig_s", [NCH, 128, D], BF16, "A")
    H1_s = b.scratch("H1_s", [NCH, 128, D], F32, "A")
    Li_s = b.scratch("Li_s", [64, TOK], F32, "A")
    Lf_s = b.scratch("Lf_s", [64, TOK], F32, "A")
    modT0_s = b.scratch("modT0_s", [128, 144, 2], F32, "A")
    if mode == "A":
        Cst_o = b.dout("Cst_o", [128, H * DVE_])
        mfin_o = b.dout("mfin_o", [64, 1])
    if mode == "B":
        C_in = b.din("C_in", [128, H * DVE_])
        m_in = b.din("m_in", [64, 1])
    c_fin0 = c_fout0 = None
    if mode == "F" and WCACHE:
        c_fin0 = b.mk_cache("c_fin0", NFC, NKC * 256)
        c_fout0 = b.mk_cache("c_fout0", NKC, NFC * 128)
        dd["c_wqk"] = b.mk_cache("c_wqk", 8, NKC * 256)
        dd["c_wtm"] = b.mk_cache("c_wtm", 10, NKC * 512)
        dd["c_wgate"] = b.mk_cache("c_wgate", 1, NKC * 128)
    if mode == "F" and USE_CC:
        sel_d = b.din("sel", [128, 2])
        cc_src = b.dram("cc_src", [128, H * DVE_ + 8], F32, "Internal")
        cc_dst = b.dram("cc_dst", [256, H * DVE_ + 8], F32, "Internal")
    if mode == "F" and not USE_CC:
        xT2_d = b.din("xT2", [NKC, 128, TOK])
        dd["ktm_o"] = b.dram("ktm_o", [NCH, 128, 1024], BF16, "Internal")
        dd["V_o"] = b.dram("V_o", [NCH, 128, H * DVE_], BF16, "Internal")
        Li_o = b.dram("Li_o", [64, TOK], F32, "Internal")
        Lf_o = b.dram("Lf_o", [64, TOK], F32, "Internal")
    if A_:
        dd["ktm_c"] = b.dram("ktm_c", [2, 128, 1024], BF16, "Internal")
        dd["V_c"] = b.dram("V_c", [2, 128, H * DVE_], BF16, "Internal")
    if B_:
        H2_s = b.dram("H2_s", [NCH, 128, D], F32, "Internal")
    for nm in ("qT", "kT", "ktmx", "Vx", "ktmc", "Vc", "ktmo", "Vo", "gates_o", "osig", "H1", "H2", "xs", "gates_s", "modT0_s", "state_o", "out"):
        dd["b_" + nm] = b.B("d_" + nm)

    b.setup_common()
    b.cst = b.sb("cst", [128, 384], F32)
    b.cstb = b.B("cst")
    b.dma(b.cst[:], cst_d, w=[b.cstb], stream="cst")
    b.identb = b.sb("identb", [128, 128], BF16)
    b.dve(lambda e: e.tensor_copy(out=b.identb[:], in_=b.cst[:, 256:384]), r=[b.cstb], w=[b.cstb])
    ng = b.sb("normg", [128, 96], F32)
    ngb = b.B("ng")
    b.dma(ng[:], normg_d, w=[ngb], stream="ng")
    mls = ExitStack()
    g = {}
    gpo = {}
    modT0 = b.sb("modT0", [128, 144, 2], F32)
    mod0b = b.B("modT0")

    def alloc_mls():
        b.cur = mls
        b.Li_x = b.sb("Li_x", [64, TOK], F32)
        b.Lf_x = b.sb("Lf_x", [64, TOK], F32)
        b.gxb = b.B("gx")
        b.C = b.sb("Cstate", [128, H * DVE_], F32)
        b.Cb_ = b.B("C")
        for nm in ("aT", "clT", "wB"):
            gpo[nm] = b.sb("gpo_" + nm, [128, NCH, 8], F32)
        gpo["b"] = b.B("gpo")
        g["b"] = b.B("g")
        for nm in ("tot", "A", "M", "mB", "wI"):
            g[nm] = b.sb("g_" + nm, [64, NCH], F32)
            b.dve(lambda e, t=g[nm]: e.memset(t[:], 0.0), w=[g["b"]])
        g["mfin"] = b.sb("g_mfin", [64, 1], F32)
        b.dve(lambda e: e.memset(g["mfin"][:], 0.0), w=[g["b"]])
        b.gp = g
        b.cur = None


    def alloc_scan():
        for nm in ("bF", "u", "aE", "cE"):
            g[nm] = b.sb("g_" + nm, [64, TOK], F32)
        s = {"q": [], "k": [], "kt": [], "V": [], "lb": [], "Ho": [], "Hob": [], "H1": [], "H1b": [],
             "St": [], "Stb": [], "dn": [], "dnb": []}
        for i in range(2):
            s["q"].append(b.sb(f"s_q{i}", [128, 1024], BF16))
            s["k"].append(b.sb(f"s_k{i}", [128, 1024], BF16))
            s["kt"].append(b.sb(f"s_kt{i}", [128, 1024], BF16))
            s["V"].append(b.sb(f"s_V{i}", [128, H * DVE_], BF16))
            s["lb"].append([b.B(f"lb{i}_{j}") for j in range(4)])
            s["Ho"].append(b.sb(f"s_Ho{i}", [128, D], F32))
            s["Hob"].append(b.B(f"Ho{i}"))
            if B_:
                s["H1"].append(b.sb(f"s_H1{i}", [128, D], F32))
                s["H1b"].append(b.B(f"H1{i}"))
            for j in range(2):
                s["St"].append(b.sb(f"s_St{i}_{j}", [128, 128], BF16))
                s["Stb"].append(b.B(f"St{i}_{j}"))
            s["dn"].append(b.sb(f"s_dn{i}", [128, 2], F32))
            s["dnb"].append(b.B(f"dn{i}"))
        s["Vt"] = b.sb("s_Vt", [128, H * DVE_], BF16)
        s["Vtb"] = b.B("Vt")
        s["Cbf"] = b.sb("s_Cbf", [128, H * DVE_], BF16)
        s["Cbfb"] = b.B("Cbf")
        b.sc = s
        b.tmp = [b.sb(f"stmp{i}", [128, NT], F32) for i in range(3)]
        b.tmpb = [b.B(f"stmp{i}") for i in range(3)]
        b.ti = 0

    if A_:
        b.bg = b.sb("bg", [64, 3], F32)
        b.bgb = b.B("bg")
        b.dma(b.bg[:, 0:2], bg_d, w=[b.bgb], stream="bg")
        b.dve(lambda e: e.tensor_scalar(out=b.bg[:, 2:3], in0=b.bg[:, 1:2], scalar1=-1.0, scalar2=None, op0=ALU.mult),
              r=[b.bgb], w=[b.bgb])
        b.Li_c = b.sb("Li_c", [64, CTX], F32)
        b.Lf_c = b.sb("Lf_c", [64, CTX], F32)
        b.gcb = b.B("gc")
        b.mod_phase(0, wmod0, bmod0, cT_d, modT0, mod0b)
        mcx0 = b.mod_consts(0, modT0, mod0b, ng, ngb, 0, "x")
        mcc0 = b.mod_consts(0, modT0, mod0b, ng, ngb, 1, "c")
        if mode == "F":
            modT1 = b.sb("modT1", [128, 144, 2], F32)
            mod1b = b.B("modT1")
            mod1_tl = b.mod_alloc(1)
            mc1_tiles = b.mod_consts_alloc(1, "x")
        alloc_mls()
        with ExitStack() as ph:
            b.cur = ph
            b.alloc_tile_state()
            b.Vst = b.sb("Vst", [128, 4, H, DVE_], BF16)
            b.Vstb = b.B("Vst")
            b.dve(lambda e: e.memset(b.Vst[:, :, :, DV:DVE_], 1.0), w=[b.Vstb])
            if mode == "F" and not USE_CC:
                for t in range(TOK // NT):
                    b.load_xT(xT2_d[:, :, t * NT:(t + 1) * NT], NT)
                    b.ffn(NT, mcx0[0], fin[0], fout[0])
                    b.mlstm_proj(NT, mcx0[1], t * 4, dd, "other")
                b.dma(Li_o, b.Li_x[:], r=[b.gxb], w=[dd["b_gates_o"]], stream="st_lio")
                b.dma(Lf_o, b.Lf_x[:], r=[b.gxb], w=[dd["b_gates_o"]], stream="st_lfo")
            for t in range(TOK // NT):
                b.load_xT(xT_d[:, :, t * NT:(t + 1) * NT], NT)
                b.ffn(NT, mcx0[0], fin[0], fout[0], c_in=c_fin0, c_out=c_fout0)
                b.store_xT(xs[:, :, t * NT:(t + 1) * NT], NT, wb=[dd["b_xs"]])
                if STOP_AT >= 0:
                    b.mlstm_proj(NT, mcx0[1], t * 4, dd, "own")
            b.load_xT(ctxT_d, CTX)
            b.ffn(CTX, mcc0[0], fin[0], fout[0], c_in=c_fin0, c_out=c_fout0)
            if STOP_AT >= 0:
                b.mlstm_proj(CTX, mcc0[1], 0, dd, "ctx")
            b.barrier()
        with ExitStack() as ph:
            b.cur = ph
            alloc_scan()
            if mode == "F":
                b.bg_gen = b.mod_phase_gen(1, wmod1, bmod1, cT_d, modT1, mod1b, mod1_tl)
            if STOP_AT >= 1:
                b.gate_prep("c", b.Li_c, b.Lf_c, b.gcb, 2, 0, "empty", gpo)
            if STOP_AT >= 2:
                b.scan(2, 0, gpo, dd, "c", True, True, False)
            b.dve(lambda e: e.tensor_copy(out=g["mB"][0:8, 0:1], in_=g["mfin"][0:8, 0:1]), r=[g["b"]], w=[g["b"]])
            if STOP_AT >= 3:
                b.gate_prep("x", b.Li_x, b.Lf_x, b.gxb, NCH, 0, "state", gpo)
            dd1 = dict(dd)
            dd1["H_wr"], dd1["b_Hout"] = H1_s, dd["b_H1"]
            if STOP_AT >= 4:
                b.scan(NCH, 0, gpo, dd1, "x", False, False, False)
            if mode == "F" and USE_CC:
                NS = H * DVE_
                sel = b.sb("sel", [128, 2], F32)
                selb = b.B("sel")
                b.dma(sel[:], sel_d, w=[selb], stream="ld_sel")
                zt = b.sb("zt", [128, NS + 8], F32)
                ztb = b.B("zt")
                b.dve(lambda e: e.memset(zt[:], 0.0), w=[ztb])
                srcb, dstb = b.B("cc_src"), b.B("cc_dst")
                b.dma(cc_src, zt[:], r=[ztb], w=[srcb], stream="cc_z")
                b.dma(cc_src[:, 0:NS], b.C[:], r=[b.Cb_], w=[srcb], stream="cc_c")
                b.dma(cc_src[0:8, NS:NS + 1], g["mfin"][0:8, 0:1], r=[g["b"]], w=[srcb], stream="cc_m", slow=True)
                b.P.add("pool", lambda e: e.collective_compute(
                    "AllGather", ALU.bypass, replica_groups=[[0, 1], [2, 3], [4, 5], [6, 7]],
                    ins=[cc_src.opt()], outs=[cc_dst.opt()]), [srcb], [dstb], dma="cc", inc=1)
                for r in range(2):
                    b.dma(zt[:], cc_dst[r * 128:(r + 1) * 128, :], r=[dstb], w=[ztb], stream="cc_ld")
                    if r == 0:
                        b.dve(lambda e: e.tensor_scalar(out=b.C[:], in0=zt[:, 0:NS], scalar1=sel[:, 0:1], scalar2=None,
                                                        op0=ALU.mult), r=[ztb, selb], w=[b.Cb_])
                    else:
                        b.dve(lambda e: e.scalar_tensor_tensor(out=b.C[:], in0=zt[:, 0:NS], scalar=sel[:, 1:2],
                                                               in1=b.C[:], op0=ALU.mult, op1=ALU.add),
                              r=[ztb, selb, b.Cb_], w=[b.Cb_])
                mt = b.sb("mt", [64, 2], F32)
                mtb = b.B("mt")
                for r in range(2):
                    b.dma(mt[32:40, r:r + 1], cc_dst[r * 128:r * 128 + 8, NS:NS + 1], r=[dstb], w=[mtb], stream=f"cc_lm{r}", slow=True)
                b.dve(lambda e: e.tensor_tensor(out=mt[32:40, :], in0=mt[32:40, :], in1=sel[32:40, :], op=ALU.mult),
                      r=[mtb, selb], w=[mtb])
                b.dve(lambda e: e.tensor_tensor(out=g["mB"][32:40, NCH - 1:NCH], in0=mt[32:40, 0:1], in1=mt[32:40, 1:2],
                                                op=ALU.add), r=[mtb, g["b"]], w=[g["b"]])
                b.gate_prep("x2", b.Li_x, b.Lf_x, b.gxb, NCH, 1, "state", gpo)
                dd2 = dict(dd)
                dd2["H_rd"], dd2["b_Hin"] = H1_s, dd["b_H1"]
                dd2["H_wr"], dd2["b_Hout"] = H2_s, dd["b_H2"]
                b.scan(NCH, 1, gpo, dd2, "x", False, False, True)
            if mode == "F" and not USE_CC:
                b.gate_prep("c1", b.Li_c, b.Lf_c, b.gcb, 2, 1, "empty", gpo)
                b.scan(2, 1, gpo, dd, "c", True, True, False)
                b.dve(lambda e: e.tensor_copy(out=g["mB"][32:40, NCH - 1:NCH], in_=g["mfin"][32:40, 0:1]),
                      r=[g["b"]], w=[g["b"]])
                Li_t = b.sb("Li_t", [64, TOK], F32)
                Lf_t = b.sb("Lf_t", [64, TOK], F32)
                gtb = b.B("gt")
                b.dma(Li_t[:], Li_o, r=[dd["b_gates_o"]], w=[gtb], stream="ld_lio")
                b.dma(Lf_t[:], Lf_o, r=[dd["b_gates_o"]], w=[gtb], stream="ld_lfo")
                b.gate_prep("o1", Li_t, Lf_t, gtb, NCH, 1, "state", gpo)
                b.scan(NCH, 1, gpo, dd, "o", True, False, False)
                b.dve(lambda e: e.tensor_copy(out=g["mB"][32:40, NCH - 1:NCH], in_=g["mfin"][32:40, 0:1]),
                      r=[g["b"]], w=[g["b"]])
                b.gate_prep("x2", b.Li_x, b.Lf_x, b.gxb, NCH, 1, "state", gpo)
                dd2 = dict(dd)
                dd2["H_rd"], dd2["b_Hin"] = H1_s, dd["b_H1"]
                dd2["H_wr"], dd2["b_Hout"] = H2_s, dd["b_H2"]
                b.scan(NCH, 1, gpo, dd2, "x", False, False, True)
            if mode == "F":
                while b.bg_gen is not None:
                    if next(b.bg_gen, "done") == "done":
                        b.bg_gen = None
                mcx1 = b.mod_consts(1, modT1, mod1b, ng, ngb, 0, "x", tiles=mc1_tiles)
            if mode == "A":
                b.dma(Cst_o, b.C[:], r=[b.Cb_], w=[dd["b_state_o"]], stream="st_C")
                b.dma(mfin_o, g["mfin"][:], r=[g["b"]], w=[dd["b_out"]], stream="st_m")
                b.dma(Li_s, b.Li_x[:], r=[b.gxb], w=[dd["b_gates_s"]], stream="st_li")
                b.dma(Lf_s, b.Lf_x[:], r=[b.gxb], w=[dd["b_modT0_s"]], stream="st_lf")
                b.dma(modT0_s, modT0[:], r=[mod0b], w=[dd["b_xs"]], stream="st_mod")
            b.barrier()
        b.cur = None
    if mode == "A":
        mls.close()
        return b.finish([dd[k] for k in dd if k.startswith("b_")])
    if mode == "B":
        b.dma(modT0[:], modT0_s, w=[mod0b], stream="ld_mod")
        mcx0 = b.mod_consts(0, modT0, mod0b, ng, ngb, 0, "x")
    if mode == "B":
        modT1 = b.sb("modT1", [128, 144, 2], F32)
        mod1b = b.B("modT1")
        b.mod_phase(1, wmod1, bmod1, cT_d, modT1, mod1b)
        mcx1 = b.mod_consts(1, modT1, mod1b, ng, ngb, 0, "x")
    if mode == "B":
        alloc_mls()
        b.dma(b.Li_x[:], Li_s, w=[b.gxb], stream="ld_li")
        b.dma(b.Lf_x[:], Lf_s, w=[b.gxb], stream="ld_lf")
        b.dma(b.C[:], C_in, w=[b.Cb_], stream="ld_C")
        b.dma(g["mB"][32:40, NCH - 1:NCH], m_in[32:40, :], w=[g["b"]], stream="ld_m")
    if mode == "B":
        with ExitStack() as ph:
            b.cur = ph
            alloc_scan()
            b.gate_prep("x2", b.Li_x, b.Lf_x, b.gxb, NCH, 1, "state", gpo)
            dd2 = dict(dd)
            dd2["H_rd"], dd2["b_Hin"] = H1_s, dd["b_H1"]
            dd2["H_wr"], dd2["b_Hout"] = H2_s, dd["b_H2"]
            b.scan(NCH, 1, gpo, dd2, "x", False, False, True)
            b.barrier()
    mls.close()
    with ExitStack() as ph:
        b.cur = ph
        b.alloc_tile_state()
        b.Hc = [b.sb(f"Hc{i}", [128, D], F32) for i in range(1)]
        b.Hcb = [b.B(f"Hc{i}") for i in range(1)]
        b.og = [b.sb(f"og{i}", [128, D], BF16) for i in range(1)]
        b.ogb = [b.B(f"og{i}") for i in range(1)]
        b.sqf = b.sb("sqf", [128, D], F32)
        b.sqfb = b.B("sqf")
        b.ss = b.sb("ss", [128, 24], F32)
        b.ssb = b.B("ss")
        b.gN = b.sb("gN", [128, D], F32)
        b.gNb = b.B("gN")
        b.dma(b.gN[:], mng_d, w=[b.gNb], stream="ld_gN")
        cw = b.sb("cw", [128, 48], F32)
        b.cw = cw[:].rearrange("p (c k) -> p c k", k=3)
        b.cwb = b.B("cw")
        b.dma(cw[:], cwT_d, w=[b.cwb], stream="ld_cw")
        fg = b.sb("fg", [128, NKC], F32)
        fgb = b.B("fg")
        b.dma(fg[:], fng_d, w=[fgb], stream="ld_fg")
        dd3 = dict(dd)
        dd3["H_fin"], dd3["b_Hout"] = H2_s, dd["b_H2"]
        for t in range(TOK // NT):
            b.load_xT(xs[:, :, t * NT:(t + 1) * NT], NT)
            b.mixer_out(NT, t * 4, mcx0[1][2], mcx0[1][3], dd3)
            b.ffn(NT, mcx0[2], fin[1], fout[1])
            b.ffn(NT, mcx1[0], fin[2], fout[2])
            b.conv_mixer(NT, mcx1[1], dd3)
            b.ffn(NT, mcx1[2], fin[3], fout[3])
            b.final_norm(NT, fg, fgb)
            b.store_xT(out_d[:, :, t * NT:(t + 1) * NT], NT, wb=[dd["b_out"]])
    b.cur = None
    return b.finish([dd["b_out"]])


def _consts():
    j = np.arange(128)[:, None]
    s_ = np.arange(128)[None, :]
    c = np.zeros((128, 384), np.float32)
    c[:, 0:128] = (j <= s_)
    c[:, 128:256] = (j >= s_)
    c[:, 256:384] = np.eye(128, dtype=np.float32)
    return c


def _prep_mlstm(inp, flipped):
    w = inp["mlstm_w_in"][0]
    bgv = inp["mlstm_b_gate"][0]
    o2 = 2 * H * DK + H * DV
    wqk = _tiles_cols(w, [np.arange(i * 256, (i + 1) * 256) for i in range(8)])
    cols = [np.arange(1024 + i * 512, 1024 + (i + 1) * 512) for i in range(2)]
    cols += [np.arange(2048 + i * 512, 2048 + (i + 1) * 512) for i in range(4)]
    cols += [np.arange(o2 + 32 + i * 512, o2 + 32 + (i + 1) * 512) for i in range(4)]
    wtm = _tiles_cols(w, cols)
    gi = [0, 2] if not flipped else [2, 0]
    gf = [1, 3] if not flipped else [3, 1]
    wg = np.zeros((D, 128), np.float32)
    bg = np.zeros((64, 2), np.float32)
    for dirn in range(2):
        r0 = 32 * dirn
        wg[:, r0:r0 + 8] = w[:, o2 + gi[dirn] * 8:o2 + gi[dirn] * 8 + 8]
        wg[:, 64 + r0:64 + r0 + 8] = w[:, o2 + gf[dirn] * 8:o2 + gf[dirn] * 8 + 8]
        bg[r0:r0 + 8, 0] = bgv[gi[dirn] * 8:gi[dirn] * 8 + 8]
        bg[r0:r0 + 8, 1] = bgv[gf[dirn] * 8:gf[dirn] * 8 + 8]
    wgate = _tiles_cols(wg, [np.arange(128)])[0]
    return wqk, wtm, wgate, bg


_CACHE = {}


def kernel(x, c, ctx, c_ctx, w_mod, b_mod, norm_g, ffn_w_in, ffn_w_out,
           mlstm_w_in, mlstm_b_gate, mlstm_norm_g, mlstm_w_out,
           conv_w_in, conv_w, conv_w_out, final_norm_g):
    inp = dict(x=x, c=c, ctx=ctx, c_ctx=c_ctx, w_mod=w_mod, b_mod=b_mod, norm_g=norm_g, ffn_w_in=ffn_w_in,
               ffn_w_out=ffn_w_out, mlstm_w_in=mlstm_w_in, mlstm_b_gate=mlstm_b_gate, mlstm_norm_g=mlstm_norm_g,
               mlstm_w_out=mlstm_w_out, conv_w_in=conv_w_in, conv_w=conv_w, conv_w_out=conv_w_out,
               final_norm_g=final_norm_g)
    inp = {k: np.asarray(v, dtype=np.float32) for k, v in inp.items()}
    sh = prep_shared(inp)
    cst = _consts()
    ml = [_prep_mlstm(inp, False), _prep_mlstm(inp, True)]
    mwout = _tiles_cols(inp["mlstm_w_out"][0], [np.arange(i * 256, (i + 1) * 256) for i in range(8)])
    mnormg = np.ascontiguousarray(np.broadcast_to(inp["mlstm_norm_g"][0][None, :], (128, D)))
    cwin = _tiles_cols(inp["conv_w_in"][0], [np.concatenate([p * D + np.arange(i * 128, (i + 1) * 128) for p in range(3)])
                                             for i in range(NKC)])
    cwout = _tiles_cols(inp["conv_w_out"][0], [np.arange(i * 256, (i + 1) * 256) for i in range(8)])
    cwT = []
    for fl in range(2):
        cwv = inp["conv_w"][0][::-1] if fl else inp["conv_w"][0]
        cwT.append(np.ascontiguousarray(np.stack([_vecT(cwv[k]) for k in range(3)], axis=-1)).reshape(128, 48))
    n = 8
    mapsA, mapsB = [], []
    for k in range(n):
        bi, s = k // 2, k % 2
        xs_ = inp["x"][bi, s * TOK:(s + 1) * TOK]
        cx = inp["ctx"][bi]
        if s:
            xs_ = xs_[::-1]
            cx = cx[::-1]
        xT = np.ascontiguousarray(xs_.T).reshape(NKC, 128, TOK)
        ctxT = np.ascontiguousarray(cx.T).reshape(NKC, 128, CTX)
        cT = np.ascontiguousarray(np.stack([_vecT(inp["c"][bi]), _vecT(inp["c_ctx"])], axis=-1))
        wqk, wtm, wgate, bg = ml[s]
        mapsA.append({"cT": cT, "normgT": sh["normgT"], "consts": cst, "xT": xT, "ctxT": ctxT,
                      "wmod0a": sh["wmod"][0][:18], "wmod0b": sh["wmod"][0][18:], "bmodT0": sh["bmodT"][0],
                      "ffn_in0": sh["ffn_in"][0], "ffn_out0": sh["ffn_out"][0],
                      "wqk": wqk, "wtm": wtm, "wgate": wgate, "bgate": bg})
        mB = {"cT": cT, "normgT": sh["normgT"], "consts": cst,
              "wmod1a": sh["wmod"][1][:18], "wmod1b": sh["wmod"][1][18:], "bmodT1": sh["bmodT"][1],
              "mwout": mwout, "mnormg": mnormg, "cwin": cwin, "cwT": cwT[s], "cwout": cwout,
              "fnormgT": sh["fnormgT"]}
        for i in (1, 2, 3):
            mB[f"ffn_in{i}"] = sh["ffn_in"][i]
            mB[f"ffn_out{i}"] = sh["ffn_out"][i]
        mapsB.append(mB)
    if FUSED:
        mapsF = []
        for k in range(n):
            bi, s = k // 2, k % 2
            xo = inp["x"][bi, (1 - s) * TOK:(2 - s) * TOK]
            if s:
                xo = xo[::-1]
            m = dict(mapsA[k])
            m.update(mapsB[k])
            if USE_CC:
                selv = np.zeros((128, 2), np.float32)
                selv[:, 1 - s] = 1.0
                m["sel"] = selv
            else:
                m["xT2"] = np.ascontiguousarray(xo.T).reshape(NKC, 128, TOK)
            mapsF.append(m)
        if "F" not in _CACHE:
            _CACHE["F"] = build_program("F")
        resF = run_bass_kernel_spmd(_CACHE["F"], mapsF, core_ids=list(range(n))).results
        out = np.empty((4, 2 * TOK, D), np.float32)
        for k in range(n):
            bi, s = k // 2, k % 2
            o = resF[k]["outT"].reshape(D, TOK).T
            if s:
                o = o[::-1]
            out[bi, s * TOK:(s + 1) * TOK] = o
        return out
    if "A" not in _CACHE:
        _CACHE["A"] = build_program("A")
        _CACHE["B"] = build_program("B")
    resA = run_bass_kernel_spmd(_CACHE["A"], mapsA, core_ids=list(range(n))).results
    for k in range(n):
        p = k ^ 1
        for nm in ("xs", "qT_s", "kT_s", "ktm_x", "V_x", "osig_s", "H1_s", "Li_s", "Lf_s", "modT0_s"):
            mapsB[k][nm] = resA[k][nm]
        mapsB[k]["C_in"] = resA[p]["Cst_o"]
        m_in = np.zeros((64, 1), np.float32)
        m_in[32:40] = resA[p]["mfin_o"][0:8]
        mapsB[k]["m_in"] = m_in
    resB = run_bass_kernel_spmd(_CACHE["B"], mapsB, core_ids=list(range(n))).results
    out = np.empty((4, 2 * TOK, D), np.float32)
    for k in range(n):
        bi, s = k // 2, k % 2
        o = resB[k]["outT"].reshape(D, TOK).T
        if s:
            o = o[::-1]
        out[bi, s * TOK:(s + 1) * TOK] = o
    return out
```
